# Optimizing a Trainium2 kernel written in Bass

```python
import jax
import jax.numpy as jnp
from jax import lax
import numpy as np

D_MODEL = 1024
BATCH = 2
SEQ = 8192
DEPTH = 2

GRID_W = 64
CTX_LEN = 256
MLA_HEADS = 8
MLA_Q_RANK = 384
MLA_KV_RANK = 256
MLA_NOPE = 64
MLA_ROPE = 32
MLA_V = 64
MLA_W = MLA_HEADS * MLA_V
NA_HEADS = 4
NA_HEAD_DIM = 64
NA_W = NA_HEADS * NA_HEAD_DIM
NA_WIN_R = 8
NA_WIN_C = 16
POOL_WINDOWS = (2, 4, 8, 16)
POOL_GROUP = 64
POOL_WIDTH = POOL_GROUP * len(POOL_WINDOWS)
N_EXPERTS = 16
EC_CAPACITY = 2
EXPERT_FF = 2048
N_BRANCH = 3
Q_BLOCK = 128
ROPE_BASE = 10000.0
EPS = 1e-6
IN_SIZES = (MLA_Q_RANK, MLA_KV_RANK, MLA_ROPE, 3 * NA_W, POOL_WIDTH, N_BRANCH * D_MODEL)
D_IN = MLA_Q_RANK + MLA_KV_RANK + MLA_ROPE + 3 * NA_W + POOL_WIDTH + N_BRANCH * D_MODEL

kernel_name = 'hybrid_mla_natten_pool_ecmoe_dit'


def rmsnorm(x, g):
    xf = x.astype(jnp.float32)
    y = xf * lax.rsqrt(jnp.mean(xf * xf, axis=-1, keepdims=True) + EPS)
    return (y * g.astype(jnp.float32)).astype(x.dtype)


def modulate(x, g, shift, scale):
    return rmsnorm(x, g) * (1 + scale) + shift


def split_in(z):
    idx = np.cumsum(IN_SIZES)[:-1].tolist()
    return jnp.split(z, idx, axis=-1)


def axial_rope(x, pos_r, pos_c):
    half = MLA_ROPE // 2
    quarter = half // 2
    inv = ROPE_BASE ** (-jnp.arange(quarter, dtype=jnp.float32) / quarter)

    def rot(xa, pos):
        ang = pos.astype(jnp.float32)[:, None] * inv
        shape = (1, ang.shape[0]) + (1,) * (xa.ndim - 3) + (quarter,)
        cos = jnp.cos(ang).reshape(shape).astype(xa.dtype)
        sin = jnp.sin(ang).reshape(shape).astype(xa.dtype)
        x1, x2 = xa[..., :quarter], xa[..., quarter:]
        return jnp.concatenate([x1 * cos - x2 * sin, x2 * cos + x1 * sin], axis=-1)

    return jnp.concatenate([rot(x[..., :half], pos_r), rot(x[..., half:], pos_c)], axis=-1)


def attend(q, k, v, scale):
    s = jnp.einsum('bqhd,bkhd->bhqk', q, k, preferred_element_type=jnp.float32) * scale
    p = jax.nn.softmax(s, axis=-1).astype(v.dtype)
    return jnp.einsum('bhqk,bkhd->bqhd', p, v)


def blocked_attend(q, k, v, scale):
    B, N, H, dq = q.shape
    qb = q.reshape(B, N // Q_BLOCK, Q_BLOCK, H, dq).transpose(1, 0, 2, 3, 4)
    ob = lax.map(lambda qi: attend(qi, k, v, scale), qb)
    return ob.transpose(1, 0, 2, 3, 4).reshape(B, N, H, v.shape[-1])


def mla_project(cq, ckv, g_q, g_kv, w_uq, w_ukv):
    B, N, _ = cq.shape
    q = (rmsnorm(cq, g_q) @ w_uq).reshape(B, N, MLA_HEADS, MLA_NOPE + MLA_ROPE)
    kv = (rmsnorm(ckv, g_kv) @ w_ukv).reshape(B, N, MLA_HEADS, MLA_NOPE + MLA_V)
    return q[..., :MLA_NOPE], q[..., MLA_NOPE:], kv[..., :MLA_NOPE], kv[..., MLA_NOPE:]


def mla_keys(k_nope, k_rope):
    kr = jnp.broadcast_to(k_rope[:, :, None, :], k_nope.shape[:-1] + (MLA_ROPE,))
    return jnp.concatenate([k_nope, kr], axis=-1)


def na_heads(z):
    B, N, _ = z.shape
    qkv = z.reshape(B, N, 3, NA_HEADS, NA_HEAD_DIM)
    return qkv[:, :, 0], qkv[:, :, 1], qkv[:, :, 2]


def na_latent(q, k, v, k_ctx, v_ctx, rpb):
    B, N, H, d = q.shape
    rows = N // GRID_W
    wr = min(NA_WIN_R, rows)
    r = jnp.arange(rows)
    start_r = jnp.clip(r - wr // 2, 0, rows - wr)
    row_idx = start_r[:, None] + jnp.arange(wr)[None, :]
    cq = jnp.arange(GRID_W)
    start_c = jnp.clip(cq - NA_WIN_C // 2, 0, GRID_W - NA_WIN_C)
    ck = jnp.arange(GRID_W)
    col_mask = (ck[None, :] >= start_c[:, None]) & (ck[None, :] < start_c[:, None] + NA_WIN_C)
    dr = row_idx - r[:, None] + (NA_WIN_R - 1)
    dc = jnp.clip(ck[None, :] - cq[:, None], -(NA_WIN_C - 1), NA_WIN_C - 1) + (NA_WIN_C - 1)
    bias = rpb[:, dr[:, None, :, None], dc[None, :, None, :]].astype(jnp.float32)
    qg = q.reshape(B, rows, GRID_W, H, d)
    kr = k.reshape(B, rows, GRID_W, H, d)[:, row_idx]
    vr = v.reshape(B, rows, GRID_W, H, d)[:, row_idx]
    scale = d ** -0.5
    s_lat = jnp.einsum('brqhd,brwkhd->bhrqwk', qg, kr, preferred_element_type=jnp.float32) * scale + bias[None]
    s_lat = jnp.where(col_mask[:, None, :], s_lat, -jnp.inf)
    s_ctx = jnp.einsum('brqhd,bchd->bhrqc', qg, k_ctx, preferred_element_type=jnp.float32) * scale
    n_lat = wr * GRID_W
    s = jnp.concatenate([s_lat.reshape(B, H, rows, GRID_W, n_lat), s_ctx], axis=-1)
    p = jax.nn.softmax(s, axis=-1).astype(v.dtype)
    p_lat = p[..., :n_lat].reshape(B, H, rows, GRID_W, wr, GRID_W)
    o = jnp.einsum('bhrqwk,brwkhd->brqhd', p_lat, vr) + jnp.einsum('bhrqc,bchd->brqhd', p[..., n_lat:], v_ctx)
    return o.reshape(B, N, H * d)


def pool_mix(u, w_pool, pool_scale):
    B, N, _ = u.shape
    uf = u.astype(jnp.float32)
    cs = jnp.concatenate([jnp.zeros((B, 1, POOL_WIDTH), jnp.float32), lax.cumsum(uf, axis=1)], axis=1)
    t = jnp.arange(N)
    outs = []
    for gi, w in enumerate(POOL_WINDOWS):
        lo = jnp.clip(t - w // 2, 0, N)
        hi = jnp.clip(t + w // 2, 0, N)
        csg = cs[..., gi * POOL_GROUP:(gi + 1) * POOL_GROUP]
        mean = (csg[:, hi] - csg[:, lo]) / (hi - lo).astype(jnp.float32)[None, :, None]
        outs.append(mean - uf[..., gi * POOL_GROUP:(gi + 1) * POOL_GROUP])
    dlt = jnp.stack(outs, axis=2).astype(u.dtype)
    y = jnp.einsum('bngc,gce->bnge', dlt, w_pool).reshape(B, N, POOL_WIDTH)
    return y * pool_scale


def merge(z_gate, a, b, p, w_br_mla, w_br_na, w_br_pool, w_out):
    B, N, _ = a.shape
    g = jax.nn.sigmoid(z_gate.astype(jnp.float32)).astype(a.dtype).reshape(B, N, N_BRANCH, D_MODEL)
    m = g[:, :, 0] * (a @ w_br_mla) + g[:, :, 1] * (b @ w_br_na) + g[:, :, 2] * (p @ w_br_pool)
    return m @ w_out


def ec_moe(h, w_router, w_gate, w_up, w_down):
    B, N, _ = h.shape
    cap = EC_CAPACITY * N // N_EXPERTS
    aff = jax.nn.softmax(jnp.einsum('bnd,de->bne', h, w_router, preferred_element_type=jnp.float32), axis=-1)
    gval, idx = lax.top_k(aff.transpose(0, 2, 1), cap)
    bidx = jnp.arange(B)[:, None, None]
    xs = h[bidx, idx]
    a = jnp.einsum('becd,edf->becf', xs, w_gate)
    u = jnp.einsum('becd,edf->becf', xs, w_up)
    y = jnp.einsum('becf,efd->becd', jax.nn.silu(a) * u, w_down)
    y = y * gval[..., None].astype(y.dtype)
    return jnp.zeros_like(h).at[bidx, idx].add(y)


def setup_inputs(seed: int = 0) -> dict:
    key = jax.random.key(seed)
    ks = jax.random.split(key, 26)
    f32 = jnp.float32

    def nrm(k, shape, scale):
        return jax.random.normal(k, shape, f32) * scale

    L, D, E, F = DEPTH, D_MODEL, N_EXPERTS, EXPERT_FF
    return {
        'x': nrm(ks[0], (BATCH, SEQ, D), 1.0),
        'c': nrm(ks[1], (BATCH, D), 1.0),
        'ctx': nrm(ks[2], (BATCH, CTX_LEN, D), 1.0),
        'c_ctx': nrm(ks[3], (D,), 1.0),
        'norm1_g': 1.0 + nrm(ks[4], (L, D), 0.02),
        'norm2_g': 1.0 + nrm(ks[5], (L, D), 0.02),
        'w_ada': nrm(ks[6], (L, D, 6 * D), 0.5 * D ** -0.5),
        'b_ada': nrm(ks[7], (L, 6 * D), 0.02),
        'w_in': nrm(ks[8], (L, D, D_IN), D ** -0.5),
        'mla_q_norm': 1.0 + nrm(ks[9], (L, MLA_Q_RANK), 0.02),
        'mla_kv_norm': 1.0 + nrm(ks[10], (L, MLA_KV_RANK), 0.02),
        'w_uq': nrm(ks[11], (L, MLA_Q_RANK, MLA_HEADS * (MLA_NOPE + MLA_ROPE)), MLA_Q_RANK ** -0.5),
        'w_ukv': nrm(ks[12], (L, MLA_KV_RANK, MLA_HEADS * (MLA_NOPE + MLA_V)), MLA_KV_RANK ** -0.5),
        'na_rpb': nrm(ks[13], (L, NA_HEADS, 2 * NA_WIN_R - 1, 2 * NA_WIN_C - 1), 0.1),
        'w_pool': nrm(ks[14], (L, len(POOL_WINDOWS), POOL_GROUP, POOL_GROUP), POOL_GROUP ** -0.5),
        'pool_scale': 1.0 + nrm(ks[15], (L, POOL_WIDTH), 0.1),
        'w_br_mla': nrm(ks[16], (L, MLA_W, D), MLA_W ** -0.5),
        'w_br_na': nrm(ks[17], (L, NA_W, D), NA_W ** -0.5),
        'w_br_pool': nrm(ks[18], (L, POOL_WIDTH, D), POOL_WIDTH ** -0.5),
        'w_out': nrm(ks[19], (L, D, D), D ** -0.5),
        'w_router': nrm(ks[20], (L, D, E), D ** -0.5),
        'w_gate': nrm(ks[21], (L, E, D, F), D ** -0.5),
        'w_up': nrm(ks[22], (L, E, D, F), D ** -0.5),
        'w_down': nrm(ks[23], (L, E, F, D), F ** -0.5),
        'final_g': 1.0 + nrm(ks[24], (D,), 0.02),
    }


def reference(x, c, ctx, c_ctx, norm1_g, norm2_g, w_ada, b_ada, w_in, mla_q_norm, mla_kv_norm, w_uq, w_ukv,
              na_rpb, w_pool, pool_scale, w_br_mla, w_br_na, w_br_pool, w_out, w_router, w_gate, w_up, w_down,
              final_g):
    B, N, _ = x.shape
    t = jnp.arange(N)
    pos_r = t // GRID_W
    pos_c = t % GRID_W
    mla_scale = (MLA_NOPE + MLA_ROPE) ** -0.5
    na_scale = NA_HEAD_DIM ** -0.5
    xl, xc = x, ctx
    for l in range(DEPTH):
        last = l == DEPTH - 1
        mod_l = (jax.nn.silu(c) @ w_ada[l] + b_ada[l])[:, None, :]
        mod_c = (jax.nn.silu(c_ctx) @ w_ada[l] + b_ada[l])[None, None, :]
        sh1l, sc1l, g1l, sh2l, sc2l, g2l = jnp.split(mod_l, 6, axis=-1)
        sh1c, sc1c, g1c, sh2c, sc2c, g2c = jnp.split(mod_c, 6, axis=-1)

        hl = modulate(xl, norm1_g[l], sh1l, sc1l)
        hc = modulate(xc, norm1_g[l], sh1c, sc1c)
        cq_l, ckv_l, kr_l, na_l, pool_l, gate_l = split_in(hl @ w_in[l])
        cq_c, ckv_c, kr_c, na_c, pool_c, gate_c = split_in(hc @ w_in[l])

        qn_l, qr_l, kn_l, v_l = mla_project(cq_l, ckv_l, mla_q_norm[l], mla_kv_norm[l], w_uq[l], w_ukv[l])
        qn_c, qr_c, kn_c, v_c = mla_project(cq_c, ckv_c, mla_q_norm[l], mla_kv_norm[l], w_uq[l], w_ukv[l])
        q_l = jnp.concatenate([qn_l, axial_rope(qr_l, pos_r, pos_c)], axis=-1)
        k_l = mla_keys(kn_l, axial_rope(kr_l, pos_r, pos_c))
        k_c = mla_keys(kn_c, kr_c)
        a_l = blocked_attend(q_l, jnp.concatenate([k_c, k_l], axis=1), jnp.concatenate([v_c, v_l], axis=1),
                             mla_scale).reshape(B, N, MLA_W)

        nq_l, nk_l, nv_l = na_heads(na_l)
        nq_c, nk_c, nv_c = na_heads(na_c)
        b_l = na_latent(nq_l, nk_l, nv_l, nk_c, nv_c, na_rpb[l])

        p_l = pool_mix(pool_l, w_pool[l], pool_scale[l])

        xl = xl + g1l * merge(gate_l, a_l, b_l, p_l, w_br_mla[l], w_br_na[l], w_br_pool[l], w_out[l])
        if not last:
            q_c = jnp.concatenate([qn_c, qr_c], axis=-1)
            a_c = attend(q_c, k_c, v_c, mla_scale).reshape(B, -1, MLA_W)
            b_c = attend(nq_c, nk_c, nv_c, na_scale).reshape(B, -1, NA_W)
            p_c = pool_mix(pool_c, w_pool[l], pool_scale[l])
            xc = xc + g1c * merge(gate_c, a_c, b_c, p_c, w_br_mla[l], w_br_na[l], w_br_pool[l], w_out[l])

        xl = xl + g2l * ec_moe(modulate(xl, norm2_g[l], sh2l, sc2l), w_router[l], w_gate[l], w_up[l], w_down[l])
        if not last:
            xc = xc + g2c * ec_moe(modulate(xc, norm2_g[l], sh2c, sc2c), w_router[l], w_gate[l], w_up[l], w_down[l])
    return rmsnorm(xl, final_g)
```

```python
import numpy as np
import concourse.bass as bass
import concourse.mybir as mybir
from concourse.bass_utils import run_bass_kernel_spmd
from contextlib import ExitStack

F32 = mybir.dt.float32
BF16 = mybir.dt.bfloat16
I32 = mybir.dt.int32
U32 = mybir.dt.uint32
ALU = mybir.AluOpType
AF = mybir.ActivationFunctionType
AX = mybir.AxisListType


class Buf:
    __slots__ = ("name", "w", "rd", "slot", "slot_phase")

    def __init__(self, name):
        self.name = name
        self.w = None
        self.rd = []
        self.slot = None
        self.slot_phase = -1


class Slot:
    __slots__ = ("sem", "total", "busy")

    def __init__(self):
        self.sem = None
        self.total = 0
        self.busy = False


class Sched:
    ENGS = ("pe", "dve", "act", "pool", "sp")

    def __init__(self, nc):
        self.nc = nc
        self.ins = {e: [] for e in self.ENGS}
        self.sig = {e: [] for e in self.ENGS}
        self.bufs = []
        self.slots = []
        self.phase = 0
        self.pending = {}
        self.last_compute = {e: None for e in self.ENGS}

    def _slot_for(self, buf):
        if buf.slot is None or buf.slot_phase != self.phase:
            s = None
            for c in self.slots:
                if not c.busy:
                    s = c
                    break
            if s is None:
                s = Slot()
                self.slots.append(s)
            s.busy = True
            buf.slot = s
            buf.slot_phase = self.phase
        return buf.slot

    def fence(self, eng):
        evs = [("d", s, s.total) for s in self.slots if s.total > 0]
        self.pending[eng] = self.pending.get(eng, []) + evs

    def barrier(self):
        evs = []
        for e in self.ENGS:
            if self.last_compute[e] is not None:
                evs.append(("e", e, self.last_compute[e]))
        for s in self.slots:
            if s.total > 0:
                evs.append(("d", s, s.total))
            s.busy = False
        self.pending = {e: list(evs) for e in self.ENGS}
        self.phase += 1

    def buf(self, name):
        b = Buf(name)
        self.bufs.append(b)
        return b

    def _add(self, eng, fn, reads, writes, dma_dst=None, inc=16):
        waits = self.pending.pop(eng, [])
        for b in reads:
            if b.w is not None:
                waits.append(b.w)
        for b in writes:
            if b.w is not None and not (b.w[0] == "e" and b.w[1] == eng):
                waits.append(b.w)
            for ev in b.rd:
                if not (ev[0] == "e" and ev[1] == eng):
                    waits.append(ev)
        idx = len(self.ins[eng])
        if dma_dst is not None:
            slot = self._slot_for(dma_dst)
            slot.total += inc
            ev = ("d", slot, slot.total)
            dma_dst = slot
        else:
            ev = ("e", eng, idx)
            self.last_compute[eng] = idx
        self.ins[eng].append([fn, waits, dma_dst, inc])
        self.sig[eng].append(False)
        for b in reads:
            b.rd.append(ev)
        for b in writes:
            b.w = ev
            b.rd = []
        return ev

    def op(self, eng, fn, reads=(), writes=()):
        return self._add(eng, fn, list(reads), list(writes))

    def dma(self, eng, fn, src, dst):
        return self._add(eng, fn, [src] if src is not None else [], [dst], dma_dst=dst)

    def finalize(self, final_bufs=()):
        nc = self.nc
        fin = []
        for b in final_bufs:
            if b.w is not None:
                fin.append(b.w)
        for s in self.slots:
            if s.total > 0:
                fin.append(("d", s, s.total))
        fin = self.pending.pop("sp", []) + fin
        self.ins["sp"].append([None, fin, None, 16])
        self.sig["sp"].append(False)
        for e in self.ENGS:
            for rec in self.ins[e]:
                pruned = []
                for ev in rec[1]:
                    if ev[0] == "e":
                        if ev[1] == e and e == "pe":
                            continue
                        self.sig[ev[1]][ev[2]] = True
                    pruned.append(ev)
                rec[1] = pruned
        cnt_at = {}
        for e in self.ENGS:
            c = 0
            arr = []
            for s in self.sig[e]:
                if s:
                    c += 1
                arr.append(c)
            cnt_at[e] = arr
        sems = {e: nc.alloc_semaphore("s_" + e) for e in self.ENGS}
        for i, s in enumerate(self.slots):
            s.sem = nc.alloc_semaphore("d_%d" % i)
        engh = {"pe": "tensor", "dve": "vector", "act": "scalar", "pool": "gpsimd", "sp": "sync"}
        with nc.Block() as block:
            for e in self.ENGS:
                def body(engine, e=e):
                    seen = {}
                    for i, (fn, waits, dmabuf, inc) in enumerate(self.ins[e]):
                        need = {}
                        for ev in waits:
                            if ev[0] == "e":
                                key = ("e", ev[1])
                                val = cnt_at[ev[1]][ev[2]]
                                sem = sems[ev[1]]
                            else:
                                key = ("d", id(ev[1]))
                                val = ev[2]
                                sem = ev[1].sem
                            if seen.get(key, 0) >= val:
                                continue
                            if key not in need or need[key][1] < val:
                                need[key] = (sem, val)
                        for key, (sem, val) in need.items():
                            engine.wait_ge(sem, val)
                            seen[key] = val
                        if fn is None:
                            continue
                        ins = fn(engine)
                        if dmabuf is not None:
                            ins.then_inc(dmabuf.sem, inc)
                        elif self.sig[e][i]:
                            ins.then_inc(sems[e], 1)
                getattr(block, engh[e])(body)


class T:
    __slots__ = ("t", "b")

    def __init__(self, t, b):
        self.t = t
        self.b = b

    def __getitem__(self, k):
        return self.t[k]


class KB:
    def __init__(self):
        self.nc = bass.Bass("TRN2", target_bir_lowering=False)
        self.S = Sched(self.nc)
        self.pid = 0
        self.stack = ExitStack()

    def end_phase(self):
        self.S.barrier()
        self.stack.close()
        self.stack = ExitStack()
        self.pid += 1

    def sb(self, name, shape, dt):
        nm = "%s_%d" % (name, self.pid)
        return T(self.stack.enter_context(self.nc.sbuf_tensor(nm, list(shape), dt)), self.S.buf(nm))

    def ps(self, name, shape=(128, 512), dt=F32):
        nm = "%s_%d" % (name, self.pid)
        return T(self.stack.enter_context(self.nc.psum_tensor(nm, list(shape), dt)), self.S.buf(nm))

    def dscr(self, name, shape, dt):
        return T(self.nc.dram_tensor(name, list(shape), dt).ap(), self.S.buf(name))

    def cc(self, kind, op, src, src_ap, dst, dst_ap):
        def fn(e):
            return e.collective_compute(kind, op, replica_groups=[[0, 1, 2, 3], [4, 5, 6, 7]],
                                        ins=[src_ap], outs=[dst_ap])
        return self.S.dma2("pool", fn, src.b, dst.b, dst.b, inc=1)

    def din(self, name, shape, dt):
        return T(self.nc.dram_tensor(name, list(shape), dt, kind="ExternalInput").ap(), self.S.buf(name))

    def dout(self, name, shape, dt):
        return T(self.nc.dram_tensor(name, list(shape), dt, kind="ExternalOutput").ap(), self.S.buf(name))

    def op(self, eng, meth, *args, r=(), w=(), **kw):
        def fn(e, meth=meth, args=args, kw=kw):
            return getattr(e, meth)(*args, **kw)
        return self.S.op(eng, fn, [x.b for x in r], [x.b for x in w])

    def dma(self, eng, out_ap, in_ap, src, dst, owner, **kw):
        def fn(e, out_ap=out_ap, in_ap=in_ap, kw=kw):
            return e.dma_start(out=out_ap, in_=in_ap, **kw)
        return self.S.dma2(eng, fn, src.b if src is not None else None, dst.b if dst is not None else None, owner.b)

    def mm(self, out_t, out_ap, lt, lhsT_ap, rt, rhs_ap, start, stop):
        return self.op("pe", "matmul", out_ap, lhsT_ap, rhs_ap, start=start, stop=stop,
                       r=[lt, rt], w=[out_t])


def _dma2(self, eng, fn, src, dst, owner, inc=16):
    reads = [src] if src is not None else []
    writes = [dst] if dst is not None else []
    if owner not in writes and owner not in reads:
        writes = writes + [owner]
    return self._add(eng, fn, reads, writes, dma_dst=owner, inc=inc)


Sched.dma2 = _dma2


D = 1024
NB = 2
NSEQ = 8192
NCTX = 256
GW = 64
NCORE = 8
TL = 2048
TT = TL + NCTX
DIN = 4768
O_CQ, O_CKV, O_KR, O_NA, O_POOL, O_GATE = 0, 384, 640, 672, 1440, 1696
EPS = 1e-6
TILES = [(0, 512), (512, 512), (1024, 512), (1536, 512), (2048, 256)]


def _rsqrt_mean(k, out_t, ss_ps, n, ncols, tmp_t):
    k.op("act", "activation", tmp_t[:, :ncols], ss_ps[:, :ncols], AF.Ln, bias=k.eps_t[:, 0:1], scale=1.0 / n,
         r=[ss_ps, k.eps_t], w=[tmp_t])
    k.op("act", "activation", out_t[:, :ncols], tmp_t[:, :ncols], AF.Exp, scale=-0.5, r=[tmp_t], w=[out_t])


def _mod_vectors(k, w_ada_l, sil, wada_s, pmod, ch0, nch):
    w_ada_v = w_ada_l.rearrange("(k p) n -> p k n", p=128)
    for bi, blk in enumerate(range(ch0 // 4, (ch0 + nch) // 4)):
        wt = wada_s[bi % 2]
        for kk in range(8):
            k.dma("sp", wt[:, kk, :], w_ada_v[:, kk, blk * 512:(blk + 1) * 512], None, wt, wt)
        for j in range(4):
            ci = bi * 4 + j
            for kk in range(8):
                k.mm(pmod, pmod[:, 2 * ci:2 * ci + 2], wt, wt[:, kk, j * 128:(j + 1) * 128], sil, sil[:, kk, :],
                     kk == 0, kk == 7)


def _common(k, io, need_id=False):
    c = {}
    c["ones"] = k.sb("ones", [128, 128], F32)
    k.eps_t = k.sb("eps", [128, 1], F32)
    k.op("dve", "memset", c["ones"][:], 1.0, w=[c["ones"]])
    k.op("dve", "memset", k.eps_t[:], EPS, w=[k.eps_t])
    if need_id:
        c["idf"] = k.sb("idf", [128, 128], F32)
        c["idb"] = k.sb("idb", [128, 128], BF16)
        k.dma("sp", c["idf"][:], io["ident"][:, :], None, c["idf"], c["idf"])
        k.op("dve", "tensor_copy", c["idb"][:], c["idf"][:], r=[c["idf"]], w=[c["idb"]])
    return c


def emit_mod(k, io, l):
    cT_s = k.sb("cT_s", [128, 8, 2], F32)
    sil = k.sb("sil", [128, 8, 2], F32)
    wada_s = [k.sb("wada0", [128, 8, 512], F32), k.sb("wada1", [128, 8, 512], F32)]
    bada_s = k.sb("bada_s", [128, 48], F32)
    mod = k.sb("modall", [128, 48, 2], F32)
    pmod = k.ps("pmod")
    k.dma("sp", cT_s[:], io["cT"][:, :, :], None, cT_s, cT_s)
    k.dma("sp", bada_s[:], io["b_adaT"].t[l], None, bada_s, bada_s)
    k.op("act", "activation", sil[:], cT_s[:], AF.Silu, r=[cT_s], w=[sil])
    _mod_vectors(k, io["w_ada"].t[l], sil, wada_s, pmod, 0, 48)
    k.op("dve", "tensor_tensor", mod[:], pmod[:, 0:96].rearrange("p (c t) -> p c t", t=2),
         bada_s[:, 0:48].unsqueeze(2).to_broadcast([128, 48, 2]), ALU.add, r=[pmod, bada_s], w=[mod])
    k.dma("sp", io["s_mod"][:, :, :], mod[:], mod, io["s_mod"], mod)
    k.end_phase()


def _mod_setup(k, io, l, ch0, nch):
    mod = k.sb("mod", [128, nch, 2], F32)
    k.dma("sp", mod[:], io["s_mod"][:, ch0:ch0 + nch, :], None, mod, mod)
    return mod


def emit_a(k, io, l, xT):
    c = _common(k, io)
    ones = c["ones"]
    o_qT, o_kT, o_v, o_nqT, o_nkT, o_nv, o_poolT, o_gateT = (io[n] for n in
        ("s_qT", "s_kT", "s_v", "s_nqT", "s_nkT", "s_nv", "s_poolT", "s_gateT"))
    win = k.sb("win", [128, 8, DIN], BF16)
    wkr_rot = k.sb("wkr_rot", [128, 8, 32], BF16)
    wuq = k.sb("wuq", [128, 3, 768], BF16)
    wuq_rot = k.sb("wuq_rot", [128, 3, 768], BF16)
    wukv = k.sb("wukv", [128, 2, 1024], BF16)
    wkn = k.sb("wkn", [128, 2, 512], BF16)
    wvv = k.sb("wvv", [128, 2, 512], BF16)
    cs = k.sb("cs", [128, 2, TL], F32)
    g1_s = k.sb("g1_s", [128, 8], F32)
    gq_s = k.sb("gq_s", [128, 3], F32)
    gkv_s = k.sb("gkv_s", [128, 2], F32)
    Acoef = k.sb("Acoef", [128, 8, 2], F32)
    xt = k.sb("xt", [128, 8, 512], F32)
    sq = k.sb("sq", [128, 512], F32)
    rstd = k.sb("rstd", [128, 512], F32)
    lntmp = k.sb("lntmp", [128, 512], F32)
    xn = k.sb("xn", [128, 512], F32)
    hT = k.sb("hT", [128, 8, TT], BF16)
    cq = k.sb("cq", [128, 3, 512], F32)
    cqn = k.sb("cqn", [128, 3, TT], BF16)
    ckv = k.sb("ckv", [128, 2, 512], F32)
    ckvn = k.sb("ckvn", [128, 2, 512], BF16)
    rt1 = k.sb("rt1", [128, 512], F32)
    rt2 = k.sb("rt2", [128, 512], F32)
    NST = 4
    st_bf = [k.sb("st_bf%d" % i, [128, 512], BF16) for i in range(NST)]
    st_f = [k.sb("st_f%d" % i, [128, 512], F32) for i in range(2)]
    pz = [k.ps("pz%d" % i) for i in range(4)]
    pss = k.ps("pss")
    pB = k.ps("pB")

    k.dma("sp", g1_s[:], io["g1T"].t[l], None, g1_s, g1_s)
    k.dma("sp", gq_s[:], io["gqT"].t[l], None, gq_s, gq_s)
    k.dma("sp", gkv_s[:], io["gkvT"].t[l], None, gkv_s, gkv_s)
    k.dma("sp", cs[0:32, :, :], io["ropeT"][:, :, :], None, cs, cs)
    k.dma("sp", cs[64:96, :, :], io["ropeT"][:, :, :], None, cs, cs)
    w_in_v = io["w_in"].t[l].rearrange("(k p) n -> p k n", p=128)
    for kk in range(8):
        for c0 in range(0, DIN, 2048):
            c1 = min(DIN, c0 + 2048)
            k.dma("pool", win[:, kk, c0:c1], w_in_v[:, kk, c0:c1], None, win, win)
    w_uq_v = io["w_uq"].t[l].rearrange("(k p) n -> p k n", p=128)
    for kk in range(3):
        k.dma("pool", wuq[:, kk, :], w_uq_v[:, kk, :], None, wuq, wuq)
    w_ukv_v = io["w_ukv"].t[l].rearrange("(k p) n -> p k n", p=128)
    for kk in range(2):
        k.dma("pool", wukv[:, kk, :], w_ukv_v[:, kk, :], None, wukv, wukv)
    wk_v = wukv[:, :, :].rearrange("p k (h r) -> p k h r", r=128)
    for kk in range(2):
        k.op("dve", "tensor_copy", wkn[:, kk, :].rearrange("p (h r) -> p h r", r=64), wk_v[:, kk, :, 0:64],
             r=[wukv], w=[wkn])
        k.op("dve", "tensor_copy", wvv[:, kk, :].rearrange("p (h r) -> p h r", r=64), wk_v[:, kk, :, 64:128],
             r=[wukv], w=[wvv])
    k.op("pool", "memset", wuq_rot[:], 0.0, w=[wuq_rot])
    for kk in range(3):
        src = wuq[:, kk, :].rearrange("p (h r) -> p h r", r=96)[:, :, 64:96].rearrange(
            "p h (a s j) -> p h a s j", a=2, s=2)
        dst = wuq_rot[:, kk, :].rearrange("p (h r) -> p h r", r=96)[:, :, 64:96].rearrange(
            "p h (a s j) -> p h a s j", a=2, s=2)
        k.op("dve", "tensor_scalar", dst[:, :, :, 0, :], src[:, :, :, 1, :], -1.0, None, ALU.mult,
             r=[wuq], w=[wuq_rot])
        k.op("dve", "tensor_copy", dst[:, :, :, 1, :], src[:, :, :, 0, :], r=[wuq], w=[wuq_rot])
    srck = win[:, :, O_KR:O_KR + 32].rearrange("p k (a s j) -> p k a s j", a=2, s=2)
    dstk = wkr_rot[:, :, :].rearrange("p k (a s j) -> p k a s j", a=2, s=2)
    k.op("dve", "tensor_scalar", dstk[:, :, :, 0, :], srck[:, :, :, 1, :], -1.0, None, ALU.mult,
         r=[win], w=[wkr_rot])
    k.op("dve", "tensor_copy", dstk[:, :, :, 1, :], srck[:, :, :, 0, :], r=[win], w=[wkr_rot])

    mod = _mod_setup(k, io, l, 0, 16)
    k.op("dve", "tensor_scalar", Acoef[:], mod[:, 8:16, :], 1.0, None, ALU.add, r=[mod], w=[Acoef])
    k.op("dve", "tensor_tensor", Acoef[:], Acoef[:], g1_s[:, :].unsqueeze(2).to_broadcast([128, 8, 2]), ALU.mult,
         r=[Acoef, g1_s], w=[Acoef])

    sti = [0]

    def store_bf(dst_t, dst_ap, src_ps_ap, src_ps, np_, ncols, eng="act", func=None):
        s_ = st_bf[sti[0] % NST]
        sti[0] += 1
        if eng == "act":
            k.op("act", "activation", s_[:np_, :ncols], src_ps_ap, func or AF.Copy, r=[src_ps], w=[s_])
        else:
            k.op("dve", "tensor_copy", s_[:np_, :ncols], src_ps_ap, r=[src_ps], w=[s_])
        k.dma("sp", dst_ap, s_[:np_, :ncols], s_, dst_t, s_)

    zi = [0]

    def nextpz():
        p = pz[zi[0] % 4]
        zi[0] += 1
        return p

    xT_v = xT.t.rearrange("(k p) n -> p k n", p=128)
    def p1a(ti, c0, ncs):
        is_ctx = ti == 4
        mi = 1 if is_ctx else 0
        for kk in range(8):
            k.dma("sp", xt[:, kk, :ncs], xT_v[:, kk, c0:c0 + ncs], None, xt, xt)
        for kk in range(8):
            k.op("act", "activation", sq[:, :ncs], xt[:, kk, :ncs], AF.Square, r=[xt], w=[sq])
            k.mm(pss, pss[:, :ncs], ones, ones[:, :], sq, sq[:, :ncs], kk == 0, kk == 7)
        _rsqrt_mean(k, rstd, pss, float(D), ncs, lntmp)
        for kk in range(8):
            k.op("dve", "tensor_tensor", xn[:, :ncs], xt[:, kk, :ncs], rstd[:, :ncs], ALU.mult, r=[xt, rstd], w=[xn])
            k.op("act", "activation", hT[:, kk, c0:c0 + ncs], xn[:, :ncs], AF.Identity,
                 bias=mod[:, kk, mi:mi + 1], scale=Acoef[:, kk, mi:mi + 1], r=[xn, mod, Acoef], w=[hT])


    def p1b(ti, c0, ncs):
        is_ctx = ti == 4
        def zchunk(col0, m):
            p = nextpz()
            for kk in range(8):
                k.mm(p, p[:m, :ncs], win, win[:, kk, col0:col0 + m], hT, hT[:, kk, c0:c0 + ncs], kk == 0, kk == 7)
            return p

        for (off, nch, raw, nrm, gs, n, nc0) in ((O_CQ, 3, cq, cqn, gq_s, 384.0, c0), (O_CKV, 2, ckv, ckvn, gkv_s, 256.0, 0)):
            for j in range(nch):
                p = zchunk(off + j * 128, 128)
                k.op("dve", "tensor_copy", raw[:, j, :ncs], p[:, :ncs], r=[p], w=[raw])
            for j in range(nch):
                k.op("act", "activation", sq[:, :ncs], raw[:, j, :ncs], AF.Square, r=[raw], w=[sq])
                k.mm(pss, pss[:, :ncs], ones, ones[:, :], sq, sq[:, :ncs], j == 0, j == nch - 1)
            _rsqrt_mean(k, rstd, pss, n, ncs, lntmp)
            for j in range(nch):
                k.op("dve", "tensor_tensor", xn[:, :ncs], raw[:, j, :ncs], rstd[:, :ncs], ALU.mult, r=[raw, rstd], w=[xn])
                k.op("act", "activation", nrm[:, j, nc0:nc0 + ncs], xn[:, :ncs], AF.Copy, scale=gs[:, j:j + 1],
                     r=[xn, gs], w=[nrm])

        p = nextpz()
        for kk in range(8):
            k.mm(p, p[:32, :ncs], win, win[:, kk, O_KR:O_KR + 32], hT, hT[:, kk, c0:c0 + ncs], kk == 0, kk == 7)
        if not is_ctx:
            for kk in range(8):
                k.mm(pB, pB[:32, :ncs], wkr_rot, wkr_rot[:, kk, :], hT, hT[:, kk, c0:c0 + ncs], kk == 0, kk == 7)
            k.op("dve", "tensor_tensor", rt1[0:32, :ncs], p[0:32, :ncs], cs[0:32, 0, c0:c0 + ncs], ALU.mult,
                 r=[p, cs], w=[rt1])
            k.op("dve", "tensor_tensor", rt2[0:32, :ncs], pB[0:32, :ncs], cs[0:32, 1, c0:c0 + ncs], ALU.mult,
                 r=[pB, cs], w=[rt2])
            s_ = st_bf[sti[0] % NST]
            sti[0] += 1
            k.op("dve", "tensor_tensor", s_[0:32, :ncs], rt1[0:32, :ncs], rt2[0:32, :ncs], ALU.add,
                 r=[rt1, rt2], w=[s_])
            k.dma("sp", o_kT[512:544, c0:c0 + ncs], s_[0:32, :ncs], s_, o_kT, s_)
        else:
            store_bf(o_kT, o_kT[512:544, c0:c0 + ncs], p[0:32, :ncs], p, 32, ncs)

        for j in range(2):
            p = zchunk(O_NA + 256 + j * 128, 128)
            store_bf(o_nkT, o_nkT[j * 128:(j + 1) * 128, c0:c0 + ncs], p[:, :ncs], p, 128, ncs, eng="dve")
        for tq in range(ncs // 128):
            p = nextpz()
            for kk in range(8):
                k.mm(p, p[:, :256], hT, hT[:, kk, c0 + tq * 128:c0 + (tq + 1) * 128], win, win[:, kk, O_NA + 512:O_NA + 768],
                     kk == 0, kk == 7)
            store_bf(o_nv, o_nv[c0 + tq * 128:c0 + (tq + 1) * 128, :], p[:, :256], p, 128, 256, eng="dve")

        for j in range(2):
            p = zchunk(O_POOL + j * 128, 128)
            s_ = st_f[j]
            k.op("dve", "tensor_copy", s_[:, :ncs], p[:, :ncs], r=[p], w=[s_])
            k.dma("sp", o_poolT[j * 128:(j + 1) * 128, c0:c0 + ncs], s_[:, :ncs], s_, o_poolT, s_)

        for hp in range(4):
            p = nextpz()
            for kk in range(2):
                k.mm(p, p[:, :ncs], wkn, wkn[:, kk, hp * 128:(hp + 1) * 128], ckvn, ckvn[:, kk, :ncs], kk == 0, kk == 1)
            store_bf(o_kT, o_kT[hp * 128:(hp + 1) * 128, c0:c0 + ncs], p[:, :ncs], p, 128, ncs, eng="dve")
        for tq in range(ncs // 128):
            p = nextpz()
            for kk in range(2):
                k.mm(p, p[:, :512], ckvn, ckvn[:, kk, tq * 128:(tq + 1) * 128], wvv, wvv[:, kk, :],
                     kk == 0, kk == 1)
            store_bf(o_v, o_v[c0 + tq * 128:c0 + (tq + 1) * 128, :], p[:, :512], p, 128, 512)


    def p2(ti, c0, ncs):
        is_ctx = ti == 4
        def zchunk(col0, m):
            p = nextpz()
            for kk in range(8):
                k.mm(p, p[:m, :ncs], win, win[:, kk, col0:col0 + m], hT, hT[:, kk, c0:c0 + ncs], kk == 0, kk == 7)
            return p

        for j in range(2):
            p = zchunk(O_NA + j * 128, 128)
            store_bf(o_nqT, o_nqT[j * 128:(j + 1) * 128, c0:c0 + ncs], p[:, :ncs], p, 128, ncs)
        for j in range(24):
            p = zchunk(O_GATE + j * 128, 128)
            store_bf(o_gateT, o_gateT[j * 128:(j + 1) * 128, c0:c0 + ncs], p[:, :ncs], p, 128, ncs, func=AF.Sigmoid)

        for h in range(8):
            p = nextpz()
            for kk in range(3):
                k.mm(p, p[:96, :ncs], wuq, wuq[:, kk, h * 96:(h + 1) * 96], cqn, cqn[:, kk, c0:c0 + ncs], kk == 0, kk == 2)
            s_ = st_bf[sti[0] % NST]
            sti[0] += 1
            if not is_ctx:
                for kk in range(3):
                    k.mm(pB, pB[:96, :ncs], wuq_rot, wuq_rot[:, kk, h * 96:(h + 1) * 96], cqn, cqn[:, kk, c0:c0 + ncs],
                         kk == 0, kk == 2)
                k.op("act", "activation", s_[0:64, :ncs], p[0:64, :ncs], AF.Copy, r=[p], w=[s_])
                k.op("dve", "tensor_tensor", rt1[64:96, :ncs], p[64:96, :ncs], cs[64:96, 0, c0:c0 + ncs], ALU.mult,
                     r=[p, cs], w=[rt1])
                k.op("dve", "tensor_tensor", rt2[64:96, :ncs], pB[64:96, :ncs], cs[64:96, 1, c0:c0 + ncs], ALU.mult,
                     r=[pB, cs], w=[rt2])
                k.op("dve", "tensor_tensor", s_[64:96, :ncs], rt1[64:96, :ncs], rt2[64:96, :ncs], ALU.add,
                     r=[rt1, rt2], w=[s_])
            else:
                k.op("act", "activation", s_[0:96, :ncs], p[0:96, :ncs], AF.Copy, r=[p], w=[s_])
            k.dma("sp", o_qT[h * 96:(h + 1) * 96, c0:c0 + ncs], s_[0:96, :ncs], s_, o_qT, s_)


    p1a(0, *TILES[0])
    for t in range(5):
        if t + 1 < 5:
            p1a(t + 1, *TILES[t + 1])
        p1b(t, *TILES[t])
        if t >= 2:
            p2(t - 2, *TILES[t - 2])

    k.S.fence("sp")
    cown = k.sb("cown", [128, 1], F32)
    nkT, nv, poolT = io["s_nkT"], io["s_nv"], io["s_poolT"]
    nkh, nvh, ph = io["s_nkh"], io["s_nvh"], io["s_ph"]
    k.dma("sp", nkh[:, 0:7 * GW], nkT[:, TL - 7 * GW:TL], nkT, nkh, cown)
    k.dma("sp", nkh[:, 7 * GW:15 * GW], nkT[:, 0:8 * GW], nkT, nkh, cown)
    k.dma("sp", nkh[:, 15 * GW:NHK], nkT[:, TL:TT], nkT, nkh, cown)
    k.dma("sp", nvh[0:7 * GW, :], nv[TL - 7 * GW:TL, :], nv, nvh, cown)
    k.dma("sp", nvh[7 * GW:15 * GW, :], nv[0:8 * GW, :], nv, nvh, cown)
    k.dma("sp", nvh[15 * GW:NHK, :], nv[TL:TT, :], nv, nvh, cown)
    k.dma("sp", ph[:, 0:HALO], poolT[:, TL - HALO:TL], poolT, ph, cown)
    k.dma("sp", ph[:, HALO:2 * HALO], poolT[:, 0:HALO], poolT, ph, cown)
    k.S.fence("pool")
    for off, cn in KT_CH:
        k.cc("AllGather", ALU.bypass, io["s_kT"], io["s_kT"][off:off + cn, :], io["g_kT"],
             io["g_kT"][4 * off:4 * off + 4 * cn, :])
    for c3 in range(3):
        k.cc("AllGather", ALU.bypass, io["s_v"], io["s_v"][c3 * V_CH:(c3 + 1) * V_CH, :], io["g_v"],
             io["g_v"][c3 * 4 * V_CH:(c3 + 1) * 4 * V_CH, :])
    for s_, g_ in (("s_nkh", "g_nkh"), ("s_nvh", "g_nvh"), ("s_ph", "g_ph")):
        k.cc("AllGather", ALU.bypass, io[s_], io[s_][:, :], io[g_], io[g_][:, :])

    p2(3, *TILES[3])
    p2(4, *TILES[4])
    k.end_phase()


NKEY = NCTX + NSEQ
NKC = NKEY // 128
MLA_SCALE = float(96 ** -0.5)
NA_SCALE = float(64 ** -0.5)
HALO = 8
FR = 47


KT_CH = [(0, 192), (192, 192), (384, 160)]
V_CH = 768
H2_CH = [(0, 512), (512, 512), (1024, 512), (1536, 512), (2048, 256)]
NHK = 7 * GW + 8 * GW + NCTX


def kt_rows(q, i0, n):
    for off, cn in KT_CH:
        if off <= i0 and i0 + n <= off + cn:
            r = 4 * off + q * cn + (i0 - off)
            return slice(r, r + n)
    raise AssertionError


def v_rows(q, t0, n):
    c = t0 // V_CH
    assert (t0 + n - 1) // V_CH == c
    r = c * 4 * V_CH + q * V_CH + (t0 - c * V_CH)
    return slice(r, r + n)


def na_window(rl):
    if rl < 4:
        return rl + 3, 6, 1 + rl
    if rl >= 29:
        return rl - 1, 6, 5 + (rl - 29)
    return rl + 3, 4, 0


def emit_frames(k, io):
    hm = k.sb("hm", [128, 8], F32)
    k.dma("sp", hm[:], io["hmask"][:, :], None, hm, hm)
    kfr, vfr, pfr = io["s_kfr"], io["s_vfr"], io["s_pfr"]
    nkT, nv, poolT = io["s_nkT"], io["s_nv"], io["s_poolT"]
    own = k.sb("own_dummy", [128, 1], F32)
    k.dma("sp", kfr[:, 7 * GW:39 * GW], nkT[:, 0:TL], nkT, kfr, own)
    k.dma("sp", vfr[7 * GW:39 * GW, :], nv[0:TL, :], nv, vfr, own)
    k.dma("sp", pfr[:, HALO:HALO + TL], poolT[:, 0:TL], poolT, pfr, own)

    def select(cand, acc, np_, base):
        k.op("dve", "tensor_scalar", acc[:], cand[:, 0], hm[:np_, base:base + 1], None, ALU.mult, r=[cand, hm], w=[acc])
        for q in range(1, 4):
            k.op("dve", "scalar_tensor_tensor", acc[:], cand[:, q], hm[:np_, base + q:base + q + 1], acc[:],
                 ALU.mult, ALU.add, r=[cand, hm, acc], w=[acc])

    nkh_g, nvh_g, ph_g = io["g_nkh"], io["g_nvh"], io["g_ph"]
    for (nm, ntok, c_lo, f_lo, base) in (("kp", 7 * GW, 0, 0, 0), ("kn", 8 * GW, 7 * GW, 39 * GW, 4)):
        cand = k.sb("cand_" + nm, [128, 4, 2, ntok], BF16)
        acc = k.sb("acc_" + nm, [128, 2, ntok], BF16)
        for q in range(4):
            k.dma("sp", cand[:, q, :, :], nkh_g[q * 256:(q + 1) * 256, c_lo:c_lo + ntok].rearrange("(c p) n -> p c n", p=128),
                  nkh_g, cand, cand)
        select(cand, acc, 128, base)
        k.dma("sp", kfr[:, f_lo:f_lo + ntok].rearrange("(c p) n -> p c n", p=128), acc[:], acc, kfr, acc)
    for (nm, nrow, r_lo, f_lo, base) in (("vp", 7, 0, 0, 0), ("vn", 8, 7, 39, 4)):
        cand = k.sb("cand_" + nm, [64, 4, nrow, 256], BF16)
        acc = k.sb("acc_" + nm, [64, nrow, 256], BF16)
        for q in range(4):
            k.dma("sp", cand[:, q, :, :],
                  nvh_g[q * NHK + r_lo * GW:q * NHK + (r_lo + nrow) * GW, :].rearrange("(a p) n -> p a n", p=64),
                  nvh_g, cand, cand)
        select(cand, acc, 64, base)
        k.dma("sp", vfr[f_lo * GW:(f_lo + nrow) * GW, :].rearrange("(a p) n -> p a n", p=64), acc[:], acc, vfr, acc)
    for (nm, c_lo, f_lo, base) in (("pp", 0, 0, 0), ("pn", HALO, HALO + TL, 4)):
        cand = k.sb("cand_" + nm, [128, 4, 2, HALO], F32)
        acc = k.sb("acc_" + nm, [128, 2, HALO], F32)
        for q in range(4):
            k.dma("sp", cand[:, q, :, :], ph_g[q * 256:(q + 1) * 256, c_lo:c_lo + HALO].rearrange("(c p) n -> p c n", p=128),
                  ph_g, cand, cand)
        select(cand, acc, 128, base)
        k.dma("sp", pfr[:, f_lo:f_lo + HALO].rearrange("(c p) n -> p c n", p=128), acc[:], acc, pfr, acc)
    zc = k.sb("zc", [128, 2, HALO], F32)
    k.op("dve", "memset", zc[:], 0.0, w=[zc])
    pcf = io["s_pcf"]
    k.dma("sp", pcf[:, 0:HALO].rearrange("(c p) n -> p c n", p=128), zc[:], zc, pcf, zc)
    k.dma("sp", pcf[:, HALO + NCTX:2 * HALO + NCTX].rearrange("(c p) n -> p c n", p=128), zc[:], zc, pcf, zc)
    k.dma("sp", pcf[:, HALO:HALO + NCTX], poolT[:, TL:TT], poolT, pcf, own)
    k.end_phase()


def emit_b1(k, io, l, with_ctx_q):
    c = _common(k, io)
    ones = c["ones"]
    qT, kT_g, v_g, nqT = io["s_qT"], io["g_kT"], io["g_v"], io["s_nqT"]
    nkh_g, nvh_g = io["g_nkh"], io["g_nvh"]
    kfr, vfr, pfr, pcf = io["s_kfr"], io["s_vfr"], io["s_pfr"], io["s_pcf"]
    o_aT, o_bT, o_pT = io["s_aT"], io["s_bT"], io["s_pT"]

    khT = [k.sb("khT%d" % i, [96, NKEY], BF16) for i in range(2)]
    vh = [k.sb("vh%d" % i, [128, NKC, 65], BF16) for i in range(2)]
    qh = [k.sb("qh%d" % i, [96, TT], BF16) for i in range(2)]
    pT = [k.sb("pT%d" % i, [128, 512], BF16) for i in range(6)]
    rrow = k.sb("rrow", [128, 512], F32)
    bcs = k.sb("bcs", [64, 512], F32)
    ost = [k.sb("ost%d" % i, [64, 512], BF16) for i in range(2)]
    NPS = 4
    ps_s = [k.ps("ps_s%d" % i) for i in range(NPS)]
    ps_o = [k.ps("ps_o%d" % i) for i in range(2)]
    ps_bc = k.ps("ps_bc")
    ps_z = k.ps("ps_z")
    for i in range(2):
        k.op("pool", "memset", vh[i][:, :, 64:65], 1.0, w=[vh[i]])

    oi = [0]

    def normalize_store(po, ncols, dst_t, dst_ap):
        k.op("dve", "reciprocal", rrow[64:65, :ncols], po[64:65, :ncols], r=[po], w=[rrow])
        k.mm(ps_bc, ps_bc[0:64, :ncols], ones, ones[64:65, 0:64], rrow, rrow[64:65, :ncols], True, True)
        k.op("act", "activation", bcs[:, :ncols], ps_bc[0:64, :ncols], AF.Copy, r=[ps_bc], w=[bcs])
        o_ = ost[oi[0] % 2]
        oi[0] += 1
        k.op("dve", "tensor_tensor", o_[:, :ncols], po[0:64, :ncols], bcs[:, :ncols], ALU.mult, r=[po, bcs], w=[o_])
        k.dma("sp", dst_ap, o_[:, :ncols], o_, dst_t, o_)

    wbd = k.sb("wbd", [128, 2, 128], BF16)
    wbd_f = k.sb("wbd_f", [128, 2, 128], F32)
    psc = k.sb("psc", [128, 2], F32)
    k.op("pool", "memset", wbd_f[:], 0.0, w=[wbd_f])
    for g in range(4):
        c_, hf = g // 2, g % 2
        k.dma("sp", wbd_f[hf * 64:(hf + 1) * 64, c_, hf * 64:(hf + 1) * 64], io["w_pool"].t[l, g], None, wbd_f, wbd_f)
    k.op("dve", "tensor_copy", wbd[:], wbd_f[:], r=[wbd_f], w=[wbd])
    k.dma("sp", psc[:], io["pscaleT"].t[l], None, psc, psc)
    LMAX = TL + 2 * HALO
    u = k.sb("pu", [128, 2, LMAX], F32)
    lv = [k.sb("plv0", [128, 2, LMAX], F32), k.sb("plv1", [128, 2, LMAX], F32)]
    cinv = k.sb("cinv", [128, 2, TT], F32)
    dlt = k.sb("dlt", [128, 2, TT], BF16)
    dtmp = k.sb("dtmp", [128, TL], F32)
    pst = [k.sb("pst%d" % i, [128, 512], BF16) for i in range(2)]
    k.dma("sp", cinv[:], io["pcinv"][:, :, :], None, cinv, cinv)
    uc = k.sb("puc", [128, 2, NCTX + 2 * HALO], F32)
    segs = [(pfr, TL, 0, u)]
    if with_ctx_q:
        segs.append((pcf, NCTX, TL, uc))
    for (src, n, col0, u) in segs:
        L = n + 2 * HALO
        k.dma("sp", u[:, :, 0:L], src.t.rearrange("(c p) n -> p c n", p=128), None, u, u)
        for g in range(4):
            c_, hf = g // 2, g % 2
            dst_t = lv[g % 2]
            k.op("pool", "memset", dst_t[:, :, 0:L], 0.0, w=[dst_t])
            if g == 0:
                k.op("pool", "tensor_tensor", dst_t[:, :, 1:L], u[:, :, 0:L - 1], u[:, :, 1:L], ALU.add, r=[u], w=[dst_t])
            else:
                pv = lv[(g - 1) % 2]
                d = (1 << g) // 2
                k.op("pool", "tensor_tensor", dst_t[:, :, d:L - d], pv[:, :, 0:L - 2 * d], pv[:, :, 2 * d:L], ALU.add,
                     r=[pv], w=[dst_t])
            ps_ = slice(hf * 64, (hf + 1) * 64)
            k.op("dve", "tensor_tensor", dtmp[ps_, 0:n], dst_t[ps_, c_, HALO:HALO + n], cinv[ps_, c_, col0:col0 + n],
                 ALU.mult, r=[dst_t, cinv], w=[dtmp])
            k.op("dve", "tensor_tensor", dlt[ps_, c_, col0:col0 + n], dtmp[ps_, 0:n], u[ps_, c_, HALO:HALO + n], ALU.subtract,
                 r=[dtmp, u], w=[dlt])

    si = [0]
    for h in range(8):
        K_ = khT[h % 2]
        V_ = vh[h % 2]
        Q_ = qh[h % 2]
        k.dma("sp", K_[0:64, 0:NCTX], kT_g[kt_rows(0, h * 64, 64), TL:TT], None, K_, K_)
        k.dma("sp", K_[64:96, 0:NCTX], kT_g[kt_rows(0, 512, 32), TL:TT], None, K_, K_)
        k.dma("sp", V_[:, 0:2, 0:64], v_g[v_rows(0, TL, NCTX), h * 64:(h + 1) * 64].rearrange("(c p) n -> p c n", p=128),
              None, V_, V_)
        for q in range(4):
            k.dma("sp", K_[0:64, NCTX + q * TL:NCTX + (q + 1) * TL], kT_g[kt_rows(q, h * 64, 64), 0:TL], None, K_, K_)
            k.dma("sp", K_[64:96, NCTX + q * TL:NCTX + (q + 1) * TL], kT_g[kt_rows(q, 512, 32), 0:TL], None, K_, K_)
            for c3 in range(3):
                t0_ = c3 * V_CH
                n_ = min(V_CH, TL - t0_)
                k.dma("sp", V_[:, 2 + q * 16 + t0_ // 128:2 + q * 16 + (t0_ + n_) // 128, 0:64],
                      v_g[v_rows(q, t0_, n_), h * 64:(h + 1) * 64].rearrange("(c p) n -> p c n", p=128), None, V_, V_)
        k.dma("sp", Q_[:, :], qT[h * 96:(h + 1) * 96, :], None, Q_, Q_)
        qtiles = [(0, 512, NKC), (512, 512, NKC), (1024, 512, NKC), (1536, 512, NKC)]
        if with_ctx_q:
            qtiles.append((2048, 256, 2))
        for (q0, nq, nkc) in qtiles:
            po = ps_o[oi[0] % 2]
            base = si[0]
            si[0] += nkc

            def s_mm(kc, K_=K_, Q_=Q_, q0=q0, nq=nq, base=base):
                s_ = ps_s[(base + kc) % NPS]
                k.mm(s_, s_[:, :nq], K_, K_[0:96, kc * 128:(kc + 1) * 128], Q_, Q_[0:96, q0:q0 + nq], True, True)

            for kc in range(min(NPS - 1, nkc)):
                s_mm(kc)
            for kc in range(nkc):
                if kc + NPS - 1 < nkc:
                    s_mm(kc + NPS - 1)
                s_ = ps_s[(base + kc) % NPS]
                p_ = pT[(base + kc) % 6]
                k.op("act", "activation", p_[:, :nq], s_[:, :nq], AF.Exp, scale=MLA_SCALE, r=[s_], w=[p_])
                k.mm(po, po[0:65, :nq], V_, V_[:, kc, 0:65], p_, p_[:, :nq], kc == 0, kc == nkc - 1)
            normalize_store(po, nq, o_aT, o_aT[h, :, q0:q0 + nq])

    eb = k.sb("eb", [128, 8, 384], F32)
    kf = k.sb("kf", [64, FR * GW], BF16)
    ve = k.sb("ve", [128, 23, 65], BF16)
    vo = k.sb("vo", [128, 23, 65], BF16)
    nkc_s = k.sb("nkc_s", [64, NCTX], BF16)
    nvc_s = k.sb("nvc_s", [128, 2, 65], BF16)
    nq_s = k.sb("nq_s", [64, TT], BF16)
    e32 = [k.sb("e32_%d" % i, [128, 512], F32) for i in range(2)]
    pb = [k.sb("pb%d" % i, [128, 512], BF16) for i in range(2)]
    k.op("pool", "memset", ve[:, :, 64:65], 1.0, w=[ve])
    k.op("pool", "memset", vo[:, :, 64:65], 1.0, w=[vo])
    k.op("pool", "memset", nvc_s[:, :, 64:65], 1.0, w=[nvc_s])
    ri = [0]
    for h in range(4):
        hs = slice(h * 64, (h + 1) * 64)
        k.dma("sp", eb[:, :, :], io["nbias"].t[l, h], None, eb, eb)
        k.op("act", "activation", eb[:], eb[:], AF.Exp, r=[eb], w=[eb])
        k.dma("sp", nkc_s[:, :], nkh_g[hs, 15 * GW:NHK], None, nkc_s, nkc_s)
        k.dma("sp", nvc_s[:, :, 0:64], nvh_g[15 * GW:NHK, hs].rearrange("(c p) n -> p c n", p=128), None, nvc_s, nvc_s)
        k.dma("sp", nq_s[:, :], nqT[hs, :], None, nq_s, nq_s)
        k.dma("sp", kf[:, :], kfr[hs, :], None, kf, kf)
        k.dma("sp", ve[:, :, 0:64], vfr[0:46 * GW, hs].rearrange("(c p) n -> p c n", p=128), None, ve, ve)
        k.dma("sp", vo[:, :, 0:64], vfr[GW:47 * GW, hs].rearrange("(c p) n -> p c n", p=128), None, vo, vo)
        nbase = si[0]
        si[0] += 32

        def na_s(rl, nbase=nbase):
            s0, nch, var = na_window(rl)
            s_ = ps_s[(nbase + rl) % NPS]
            qcols = nq_s[:, rl * 64:(rl + 1) * 64]
            for i in range(nch):
                k.mm(s_, s_[:, i * 64:(i + 1) * 64], kf, kf[:, (s0 + 2 * i) * GW:(s0 + 2 * i + 2) * GW], nq_s, qcols,
                     True, True)
            for c_ in range(2):
                k.mm(s_, s_[:, (nch + c_) * 64:(nch + c_ + 1) * 64], nkc_s, nkc_s[:, c_ * 128:(c_ + 1) * 128], nq_s, qcols,
                     True, True)

        na_s(0)
        na_s(1)
        for rl in range(32):
            rg, r8 = rl // 8, rl % 8
            if r8 == 0:
                po = ps_o[oi[0] % 2]
            if rl + 2 < 32:
                na_s(rl + 2)
            s0, nch, var = na_window(rl)
            s_ = ps_s[(nbase + rl) % NPS]
            nl = nch * 64
            e_ = e32[ri[0] % 2]
            b_ = pb[ri[0] % 2]
            ri[0] += 1
            k.op("act", "activation", e_[:, 0:nl + 128], s_[:, 0:nl + 128], AF.Exp, scale=NA_SCALE, r=[s_], w=[e_])
            k.op("dve", "tensor_tensor", b_[:, 0:nl], e_[:, 0:nl], eb[:, var, 0:nl], ALU.mult, r=[e_, eb], w=[b_])
            k.op("pool", "tensor_copy", b_[:, nl:nl + 128], e_[:, nl:nl + 128], r=[e_], w=[b_])
            vt = ve if s0 % 2 == 0 else vo
            c0_ = s0 // 2
            for i in range(nch):
                k.mm(po, po[0:65, r8 * 64:(r8 + 1) * 64], vt, vt[:, c0_ + i, 0:65], b_, b_[:, i * 64:(i + 1) * 64],
                     i == 0, False)
            for c_ in range(2):
                k.mm(po, po[0:65, r8 * 64:(r8 + 1) * 64], nvc_s, nvc_s[:, c_, 0:65], b_,
                     b_[:, (nch + c_) * 64:(nch + c_ + 1) * 64], False, c_ == 1)
            if r8 == 7:
                normalize_store(po, 512, o_bT, o_bT[h, :, rg * 512:(rg + 1) * 512])
        if with_ctx_q:
            po = ps_o[oi[0] % 2]
            for c_ in range(2):
                s_ = ps_s[si[0] % NPS]
                p_ = pT[si[0] % 6]
                si[0] += 1
                k.mm(s_, s_[:, :256], nkc_s, nkc_s[:, c_ * 128:(c_ + 1) * 128], nq_s, nq_s[:, 2048:2304], True, True)
                k.op("act", "activation", p_[:, :256], s_[:, :256], AF.Exp, scale=NA_SCALE, r=[s_], w=[p_])
                k.mm(po, po[0:65, :256], nvc_s, nvc_s[:, c_, 0:65], p_, p_[:, :256], c_ == 0, c_ == 1)
            normalize_store(po, 256, o_bT, o_bT[h, :, 2048:2304])

    for (src, n, col0, u_) in segs:
        for c_ in range(2):
            for t0 in range(0, n, 512):
                nn = min(512, n - t0)
                k.mm(ps_z, ps_z[:, :nn], wbd, wbd[:, c_, :], dlt, dlt[:, c_, col0 + t0:col0 + t0 + nn], True, True)
                s_ = pst[oi[0] % 2]
                oi[0] += 1
                k.op("act", "activation", s_[:, :nn], ps_z[:, :nn], AF.Copy, scale=psc[:, c_:c_ + 1], r=[ps_z, psc], w=[s_])
                k.dma("sp", o_pT[c_ * 128:(c_ + 1) * 128, col0 + t0:col0 + t0 + nn], s_[:, :nn], s_, o_pT, s_)


    k.end_phase()


def emit_b2(k, io, l, xT, with_ctx):
    c = _common(k, io, need_id=True)
    ones, idb = c["ones"], c["idb"]
    aT, bT, pT, gateT = io["s_aT"], io["s_bT"], io["s_pT"], io["s_gateT"]
    o_xmidT, o_h2, o_aff = io["s_xmidT"], io["s_h2"], io["s_aff"]
    wmla = k.sb("wmla", [64, 8, D], BF16)
    wna = k.sb("wna", [64, 4, D], BF16)
    wpl = k.sb("wpl", [128, 2, D], BF16)
    wout = k.sb("wout", [128, 8, D], BF16)
    wr = k.sb("wr", [128, 8, 16], F32)
    g2_s = k.sb("g2_s", [128, 8], F32)
    Acoef = k.sb("Acoef", [128, 8, 2], F32)
    a_t = k.sb("a_t", [64, 8, 512], BF16)
    b_t = k.sb("b_t", [64, 4, 512], BF16)
    p_t = k.sb("p_t", [128, 2, 512], BF16)
    g_t = k.sb("g_t", [128, 24, 512], BF16)
    xt = k.sb("xt", [128, 8, 512], F32)
    mT = k.sb("mT", [128, 8, 512], BF16)
    xmid = k.sb("xmid", [128, 8, 512], F32)
    h2f = k.sb("h2f", [128, 8, 512], F32)
    h2b = k.sb("h2b", [128, 8, 512], BF16)
    t1 = k.sb("t1", [128, 512], F32)
    t2 = k.sb("t2", [128, 512], F32)
    sq = k.sb("sq", [128, 512], F32)
    rstd = k.sb("rstd", [128, 512], F32)
    lntmp = k.sb("lntmp", [128, 512], F32)
    xn = k.sb("xn", [128, 512], F32)
    htok = [k.sb("htok%d" % i, [128, D], BF16) for i in range(2)]
    afft = [k.sb("afft%d" % i, [128, 16], F32) for i in range(2)]
    sm = k.sb("sm", [128, 8], F32)
    ex = k.sb("ex", [128, 16], F32)
    pa = k.ps("pa")
    pb_ = k.ps("pb")
    pp = k.ps("pp")
    po = [k.ps("po0"), k.ps("po1")]
    pss = k.ps("pss")
    ptr = k.ps("ptr", [128, D], BF16)

    zt = k.sb("zt", [128, 2, D], F32)
    k.op("pool", "memset", zt[:], 0.0, w=[zt])
    yp_v = io["s_ypart"].t.rearrange("(n p) d -> p n d", p=128)
    for i in range(0, 4 * TT // 128, 2):
        k.dma("act", yp_v[:, i:i + 2, :], zt[:, :, :], zt, io["s_ypart"], zt)
    k.dma("sp", g2_s[:], io["g2T"].t[l], None, g2_s, g2_s)
    k.dma("sp", wr[:], io["w_router"].t[l].rearrange("(k p) n -> p k n", p=128), None, wr, wr)
    wm_v = io["w_br_mla"].t[l].rearrange("(h p) n -> p h n", p=64)
    for h in range(8):
        k.dma("pool", wmla[:, h, :], wm_v[:, h, :], None, wmla, wmla)
    wn_v = io["w_br_na"].t[l].rearrange("(h p) n -> p h n", p=64)
    for h in range(4):
        k.dma("pool", wna[:, h, :], wn_v[:, h, :], None, wna, wna)
    wp_v = io["w_br_pool"].t[l].rearrange("(k p) n -> p k n", p=128)
    for kk in range(2):
        k.dma("pool", wpl[:, kk, :], wp_v[:, kk, :], None, wpl, wpl)
    wo_v = io["w_out"].t[l].rearrange("(k p) n -> p k n", p=128)
    for kk in range(8):
        k.dma("pool", wout[:, kk, :], wo_v[:, kk, :], None, wout, wout)
    mod = _mod_setup(k, io, l, 16, 24)
    k.op("dve", "tensor_scalar", Acoef[:], mod[:, 16:24, :], 1.0, None, ALU.add, r=[mod], w=[Acoef])
    k.op("dve", "tensor_tensor", Acoef[:], Acoef[:], g2_s[:, :].unsqueeze(2).to_broadcast([128, 8, 2]), ALU.mult,
         r=[Acoef, g2_s], w=[Acoef])

    hi = [0]
    tiles = ([TILES[4]] + TILES[:4]) if with_ctx else TILES[:4]
    xT_v = xT.t.rearrange("(k p) n -> p k n", p=128)
    g_v = gateT.t.rearrange("(k p) n -> p k n", p=128)
    for (c0, ncs) in tiles:
        mi = 1 if c0 == TL else 0
        k.dma("sp", a_t[:, :, :ncs], aT.t[:, :, c0:c0 + ncs].rearrange("h p n -> p h n"), None, a_t, a_t)
        k.dma("sp", b_t[:, :, :ncs], bT.t[:, :, c0:c0 + ncs].rearrange("h p n -> p h n"), None, b_t, b_t)
        k.dma("sp", p_t[:, :, :ncs], pT.t.rearrange("(k p) n -> p k n", p=128)[:, :, c0:c0 + ncs], None, p_t, p_t)
        for q in range(3):
            k.dma("sp", g_t[:, q * 8:(q + 1) * 8, :ncs], g_v[:, q * 8:(q + 1) * 8, c0:c0 + ncs], None, g_t, g_t)
        for kk in range(8):
            k.dma("sp", xt[:, kk, :ncs], xT_v[:, kk, c0:c0 + ncs], None, xt, xt)
        for j in range(8):
            js = slice(j * 128, (j + 1) * 128)
            for h in range(8):
                k.mm(pa, pa[:, :ncs], wmla, wmla[:, h, js], a_t, a_t[:, h, :ncs], h == 0, h == 7)
            for h in range(4):
                k.mm(pb_, pb_[:, :ncs], wna, wna[:, h, js], b_t, b_t[:, h, :ncs], h == 0, h == 3)
            for kk in range(2):
                k.mm(pp, pp[:, :ncs], wpl, wpl[:, kk, js], p_t, p_t[:, kk, :ncs], kk == 0, kk == 1)
            k.op("dve", "tensor_tensor", t1[:, :ncs], pa[:, :ncs], g_t[:, j, :ncs], ALU.mult, r=[pa, g_t], w=[t1])
            k.op("dve", "tensor_tensor", t2[:, :ncs], pb_[:, :ncs], g_t[:, 8 + j, :ncs], ALU.mult, r=[pb_, g_t], w=[t2])
            k.op("pool", "tensor_tensor", t1[:, :ncs], t1[:, :ncs], t2[:, :ncs], ALU.add, r=[t1, t2], w=[t1])
            k.op("dve", "tensor_tensor", t2[:, :ncs], pp[:, :ncs], g_t[:, 16 + j, :ncs], ALU.mult, r=[pp, g_t], w=[t2])
            k.op("pool", "tensor_tensor", mT[:, j, :ncs], t1[:, :ncs], t2[:, :ncs], ALU.add, r=[t1, t2], w=[mT])
        for i in range(8):
            p_ = po[i % 2]
            for j in range(8):
                k.mm(p_, p_[:, :ncs], wout, wout[:, j, i * 128:(i + 1) * 128], mT, mT[:, j, :ncs], j == 0, j == 7)
            k.op("dve", "scalar_tensor_tensor", xmid[:, i, :ncs], p_[:, :ncs], mod[:, i, mi:mi + 1], xt[:, i, :ncs],
                 ALU.mult, ALU.add, r=[p_, mod, xt], w=[xmid])
        k.dma("sp", o_xmidT.t.rearrange("(k p) n -> p k n", p=128)[:, :, c0:c0 + ncs], xmid[:, :, :ncs], xmid, o_xmidT, xmid)
        for kk in range(8):
            k.op("act", "activation", sq[:, :ncs], xmid[:, kk, :ncs], AF.Square, r=[xmid], w=[sq])
            k.mm(pss, pss[:, :ncs], ones, ones[:, :], sq, sq[:, :ncs], kk == 0, kk == 7)
        _rsqrt_mean(k, rstd, pss, float(D), ncs, lntmp)
        for kk in range(8):
            k.op("dve", "tensor_tensor", xn[:, :ncs], xmid[:, kk, :ncs], rstd[:, :ncs], ALU.mult, r=[xmid, rstd], w=[xn])
            k.op("act", "activation", h2f[:, kk, :ncs], xn[:, :ncs], AF.Identity,
                 bias=mod[:, 8 + kk, mi:mi + 1], scale=Acoef[:, kk, mi:mi + 1], r=[xn, mod, Acoef], w=[h2f])
        k.op("pool", "tensor_copy", h2b[:, :, :ncs], h2f[:, :, :ncs], r=[h2f], w=[h2b])
        for tq in range(ncs // 128):
            ts_ = slice(tq * 128, (tq + 1) * 128)
            lg = po[tq % 2]
            for kk in range(8):
                k.mm(lg, lg[:, 0:16], h2f, h2f[:, kk, ts_], wr, wr[:, kk, :], kk == 0, kk == 7)
            k.op("dve", "tensor_reduce", sm[:, 0:1], lg[:, 0:16], AX.X, ALU.max, r=[lg], w=[sm])
            k.op("dve", "tensor_scalar", sm[:, 1:2], sm[:, 0:1], -1.0, None, ALU.mult, r=[sm], w=[sm])
            k.op("act", "activation", ex[:, :], lg[:, 0:16], AF.Exp, bias=sm[:, 1:2], scale=1.0, r=[lg, sm], w=[ex])
            k.op("dve", "tensor_reduce", sm[:, 2:3], ex[:, :], AX.X, ALU.add, r=[ex], w=[sm])
            k.op("dve", "reciprocal", sm[:, 3:4], sm[:, 2:3], r=[sm], w=[sm])
            af_ = afft[hi[0] % 2]
            k.op("dve", "tensor_scalar", af_[:, :], ex[:, :], sm[:, 3:4], None, ALU.mult, r=[ex, sm], w=[af_])
            k.dma("sp", o_aff[c0 + tq * 128:c0 + (tq + 1) * 128, :], af_[:, :], af_, o_aff, af_)
            for kk in range(8):
                k.op("pe", "transpose", ptr[:, kk * 128:(kk + 1) * 128], h2b[:, kk, ts_], idb[:, :], r=[h2b, idb], w=[ptr])
            ht_ = htok[hi[0] % 2]
            hi[0] += 1
            k.op("act", "activation", ht_[:, :], ptr[:, :], AF.Copy, r=[ptr], w=[ht_])
            k.dma("sp", o_h2[c0 + tq * 128:c0 + (tq + 1) * 128, :], ht_[:, :], ht_, o_h2, ht_)
        k.S.fence("pool")
        k.cc("AllGather", ALU.bypass, o_h2, o_h2[c0:c0 + ncs, :], io["g_h2"], io["g_h2"][4 * c0:4 * c0 + 4 * ncs, :])
    k.end_phase()


FF = 2048
NBIS = 27
NE = 4


def emit_c(k, io, l, with_ctx):
    c = _common(k, io, need_id=True)
    ones, idb = c["ones"], c["idb"]
    h2_g, aff_g, ypart = io["g_h2"], io["g_aff"], io["s_ypart"]
    wg = k.sb("wg", [128, 8, FF], BF16)
    wu = k.sb("wu", [128, 8, FF], BF16)
    wd = k.sb("wd", [128, 16, D], BF16)
    xsT = k.sb("xsT", [128, 8, 1152], BF16)
    hdn = k.sb("hdn", [128, 16, 1152], BF16)
    ones64 = k.sb("ones64", [128, 64], F32)
    iot = k.sb("iot", [128, 1024], F32)
    pj = k.sb("pj", [128, 64, 4], F32)
    pjc = k.sb("pjc", [128, 2, 4], F32)
    trif = k.sb("trif", [128, 128], F32)
    trib = k.sb("trib", [128, 128], BF16)
    cbase = k.sb("cbase", [128, 2], F32)
    esel = k.sb("esel", [128, NE, 16], F32)
    Aall = k.sb("Aall", [128, 64, 16], F32)
    Acall = k.sb("Acall", [128, 2, 16], F32)
    Atmp = k.sb("Atmp", [128, 64, 16], F32)
    A = k.sb("A", [128, NE, 64], F32)
    Ac = k.sb("Ac", [128, NE, 2], F32)
    cmpL = k.sb("cmpL", [128, NE, 64], F32)
    cmpC = k.sb("cmpC", [128, NE, 2], F32)
    lo = k.sb("lo", [128, 8], F32)
    mid = k.sb("mid", [128, 8], F32)
    cnt = k.sb("cnt", [128, 8], F32)
    fs = k.sb("fs", [128, 8], F32)
    incl = k.sb("incl", [128, 64], F32)
    rowt = k.sb("rowt", [128, 1], BF16)
    offs = k.sb("offs", [128, 1], F32)
    key = k.sb("key", [128, 64], F32)
    R = k.sb("R", [128, 64, 7], BF16)
    r1 = k.sb("r1", [128, 64], F32)
    r2 = k.sb("r2", [128, 64], F32)
    OH = [k.sb("OH%d" % i, [128, 1024], BF16) for i in range(4)]
    sel = k.sb("sel", [128, 9, 8], F32)
    idxf = k.sb("idxf", [128, 9], F32)
    idx2 = k.sb("idx2", [128, 9], F32)
    idxi_all = k.sb("idxi", [128, NE, 9], I32)
    idxs_all = k.sb("idxs", [128, NE, 9], I32)
    gval_all = k.sb("gval", [128, NE, 9], F32)
    xs_tok = [k.sb("xs_tok%d" % i, [128, D], BF16) for i in range(2)]
    sg = [k.sb("sg%d" % i, [128, 512], F32) for i in range(2)]
    y_sb = [k.sb("y_sb%d" % i, [128, D], F32) for i in range(2)]
    prt = k.ps("prt")
    ptot = prt
    poff = prt
    psel = prt
    ptr = k.ps("ptr", [128, D], BF16)
    pa2 = [k.ps("pa0"), k.ps("pa1")]
    pu2 = [k.ps("pu0"), k.ps("pu1")]
    py = [k.ps("py0"), k.ps("py1")]

    k.op("dve", "memset", ones64[:], 1.0, w=[ones64])
    k.dma("sp", iot[:], io["c_iota"][:, :], None, iot, iot)
    k.dma("sp", pj[:], io["c_pj"][:, :, :], None, pj, pj)
    k.dma("sp", pjc[:], io["c_pjc"][:, :, :], None, pjc, pjc)
    k.dma("sp", trif[:], io["c_tri"][:, :], None, trif, trif)
    k.dma("sp", cbase[:], io["c_base"][:, :], None, cbase, cbase)
    k.dma("sp", esel[:], io["esel"][:, :, :], None, esel, esel)
    k.op("dve", "tensor_copy", trib[:], trif[:], r=[trif], w=[trib])
    for q in range(4):
        k.dma("sp", Aall[:, q * 16:(q + 1) * 16, :], aff_g[q * TT:q * TT + TL, :].rearrange("(p j) e -> p j e", j=16),
              None, Aall, Aall)
    if with_ctx:
        k.dma("sp", Acall[:, :, :], aff_g[TL:TT, :].rearrange("(p j) e -> p j e", j=2), None, Acall, Acall)
    else:
        k.op("dve", "memset", Acall[:], 0.0, w=[Acall])
    for e in range(NE):
        k.op("dve", "tensor_tensor", Atmp[:], Aall[:], esel[:, e, :].unsqueeze(1).to_broadcast([128, 64, 16]), ALU.mult,
             r=[Aall, esel], w=[Atmp])
        k.op("dve", "tensor_reduce", A[:, e, :], Atmp[:], AX.X, ALU.add, r=[Atmp], w=[A])
        k.op("dve", "tensor_tensor", Atmp[:, 0:2, :], Acall[:], esel[:, e, :].unsqueeze(1).to_broadcast([128, 2, 16]),
             ALU.mult, r=[Acall, esel], w=[Atmp])
        k.op("dve", "tensor_reduce", Ac[:, e, :], Atmp[:, 0:2, :], AX.X, ALU.add, r=[Atmp], w=[Ac])

    k.op("dve", "memset", lo[:], 0.0, w=[lo])
    for it in range(1, NBIS + 1):
        step = float(2.0 ** -it)
        k.op("dve", "tensor_scalar", mid[:], lo[:], step, None, ALU.add, r=[lo], w=[mid])
        k.op("dve", "tensor_tensor", cmpL[:], A[:], mid[:, 0:4].unsqueeze(2).to_broadcast([128, 4, 64]), ALU.is_ge,
             r=[A, mid], w=[cmpL])
        k.op("dve", "tensor_tensor", cmpC[:], Ac[:], mid[:, 4:8].unsqueeze(2).to_broadcast([128, 4, 2]), ALU.is_ge,
             r=[Ac, mid], w=[cmpC])
        k.op("dve", "tensor_reduce", cnt[:, 0:4], cmpL[:], AX.X, ALU.add, r=[cmpL], w=[cnt])
        k.op("dve", "tensor_reduce", cnt[:, 4:8], cmpC[:], AX.X, ALU.add, r=[cmpC], w=[cnt])
        k.mm(ptot, ptot[:, 96:104], ones, ones[:, :], cnt, cnt[:, :], True, True)
        k.op("dve", "tensor_scalar", fs[:, 0:4], ptot[:, 96:100], 1023.5, step, ALU.is_ge, ALU.mult, r=[ptot], w=[fs])
        k.op("dve", "tensor_scalar", fs[:, 4:8], ptot[:, 100:104], 31.5, step, ALU.is_ge, ALU.mult, r=[ptot], w=[fs])
        k.op("dve", "tensor_tensor", lo[:], lo[:], fs[:], ALU.add, r=[lo, fs], w=[lo])

    ohi = [0]
    xi = [0]
    yi = [0]

    def compact(avals, pj_t, thr_col, ncol, nslot_tiles, nsl, st0):
        k.op("dve", "tensor_scalar", key[:, :ncol], avals, lo[:, thr_col:thr_col + 1], None, ALU.is_ge,
             r=[A, Ac, lo], w=[key])
        k.op("dve", "tensor_tensor_scan", incl[:, :ncol], ones64[:, :ncol], key[:, :ncol], 0.0, ALU.mult, ALU.add,
             r=[ones64, key], w=[incl])
        k.op("dve", "tensor_copy", rowt[:, :], incl[:, ncol - 1:ncol], r=[incl], w=[rowt])
        k.mm(poff, poff[:, 112:113], trib, trib[:, :], rowt, rowt[:, :], True, True)
        k.op("dve", "tensor_copy", offs[:, :], poff[:, 112:113], r=[poff], w=[offs])
        k.op("dve", "tensor_scalar", incl[:, :ncol], incl[:, :ncol], offs[:, 0:1], None, ALU.add, r=[incl, offs], w=[incl])
        k.op("dve", "tensor_tensor", key[:, :ncol], key[:, :ncol], incl[:, :ncol], ALU.mult, r=[key, incl], w=[key])
        k.op("dve", "tensor_copy", R[:, :ncol, 0:4], pj_t[:, :ncol, :], r=[pj_t], w=[R])
        k.op("dve", "tensor_copy", R[:, :ncol, 4], avals, r=[A, Ac], w=[R])
        k.op("dve", "tensor_tensor", r1[:, :ncol], avals, R[:, :ncol, 4], ALU.subtract, r=[A, Ac, R], w=[r1])
        k.op("dve", "tensor_copy", R[:, :ncol, 5], r1[:, :ncol], r=[r1], w=[R])
        k.op("dve", "tensor_tensor", r2[:, :ncol], r1[:, :ncol], R[:, :ncol, 5], ALU.subtract, r=[r1, R], w=[r2])
        k.op("dve", "tensor_copy", R[:, :ncol, 6], r2[:, :ncol], r=[r2], w=[R])
        for j in range(ncol):
            oh = OH[ohi[0] % 4]
            ohi[0] += 1
            if nsl < 128:
                k.op("pool", "memset", oh[:, 0:128], 0.0, w=[oh])
            k.op("dve", "tensor_scalar", oh[:, 0:nsl], iot[:, 0:nsl], key[:, j:j + 1], None, ALU.is_equal,
                 r=[iot, key], w=[oh])
            for st in range(nslot_tiles):
                k.op("pe", "matmul", psel[:, (st0 + st) * 8:(st0 + st) * 8 + 7], oh[:, st * 128:(st + 1) * 128],
                     R[:, j, :], start=(j == 0 and st == 0), stop=(j == ncol - 1), skip_group_check=True,
                     r=[oh, R], w=[psel])

    def finish_sel(e, st0, nst, mg, ms, base_col):
        k.op("dve", "tensor_copy", sel[:, st0:st0 + nst, :], psel[:, st0 * 8:(st0 + nst) * 8].rearrange("p (s c) -> p s c", c=8), r=[psel], w=[sel])
        for (mults, dst_i, bcol) in ((mg, idxi_all, 0), (ms, idxs_all, 1)):
            k.op("dve", "tensor_scalar", idxf[:, st0:st0 + nst], sel[:, st0:st0 + nst, 0], float(mults[0]), None, ALU.mult, r=[sel], w=[idxf])
            for ci in (1, 2):
                k.op("dve", "scalar_tensor_tensor", idxf[:, st0:st0 + nst], sel[:, st0:st0 + nst, ci], float(mults[ci]), idxf[:, st0:st0 + nst],
                     ALU.mult, ALU.add, r=[sel, idxf], w=[idxf])
            k.op("dve", "tensor_tensor", idxf[:, st0:st0 + nst], idxf[:, st0:st0 + nst], sel[:, st0:st0 + nst, 3], ALU.add, r=[idxf, sel], w=[idxf])
            if base_col:
                k.op("dve", "tensor_scalar", idxf[:, st0:st0 + nst], idxf[:, st0:st0 + nst], cbase[:, bcol:bcol + 1], None, ALU.add,
                     r=[idxf, cbase], w=[idxf])
            k.op("dve", "tensor_copy", dst_i[:, e, st0:st0 + nst], idxf[:, st0:st0 + nst], r=[idxf], w=[dst_i])
        k.op("dve", "tensor_tensor", gval_all[:, e, st0:st0 + nst], sel[:, st0:st0 + nst, 4], sel[:, st0:st0 + nst, 5], ALU.add, r=[sel], w=[gval_all])
        k.op("dve", "tensor_tensor", gval_all[:, e, st0:st0 + nst], gval_all[:, e, st0:st0 + nst], sel[:, st0:st0 + nst, 6], ALU.add, r=[gval_all, sel], w=[gval_all])

    def gather_T(ex, st0, nst):
        for st in range(st0, st0 + nst):
            x_ = xs_tok[xi[0] % 2]
            xi[0] += 1

            def fn(e, x_=x_, st=st):
                return e.indirect_dma_start(out=x_[:, :], out_offset=None, in_=h2_g.t[:, :],
                                            in_offset=bass.IndirectOffsetOnAxis(ap=idxi_all[:, ex, st:st + 1], axis=0))
            k.S.dma2("pool", fn, idxi_all.b, x_.b, x_.b)
            for kk in range(8):
                k.op("pe", "transpose", ptr[:, kk * 128:(kk + 1) * 128], x_[:, kk * 128:(kk + 1) * 128], idb[:, :],
                     r=[x_, idb], w=[ptr])
            k.op("act", "activation", xsT[:, :, st * 128:(st + 1) * 128], ptr[:, :].rearrange("p (k s) -> p k s", s=128),
                 AF.Copy, r=[ptr], w=[xsT])

    first_scatter = [True]

    def gu(nst):
        ncols = nst * 128
        for fc in range(16):
            fs_ = slice(fc * 128, (fc + 1) * 128)
            for c0 in range(0, ncols, 512):
                nn = min(512, ncols - c0)
                pa = pa2[yi[0] % 2]
                pu = pu2[yi[0] % 2]
                for kk in range(8):
                    k.mm(pa, pa[:, :nn], wg, wg[:, kk, fs_], xsT, xsT[:, kk, c0:c0 + nn], kk == 0, kk == 7)
                for kk in range(8):
                    k.mm(pu, pu[:, :nn], wu, wu[:, kk, fs_], xsT, xsT[:, kk, c0:c0 + nn], kk == 0, kk == 7)
                s_ = sg[yi[0] % 2]
                yi[0] += 1
                k.op("act", "activation", s_[:, :nn], pa[:, :nn], AF.Silu, r=[pa], w=[s_])
                k.op("dve", "tensor_tensor", hdn[:, fc, c0:c0 + nn], pu[:, :nn], s_[:, :nn], ALU.mult, r=[pu, s_], w=[hdn])

    def down(ex, nst):
        for st in range(nst):
            y_ = y_sb[yi[0] % 2]
            yi[0] += 1
            for dh in range(2):
                p_ = py[dh]
                for fc in range(16):
                    k.mm(p_, p_[:, :512], hdn, hdn[:, fc, st * 128:(st + 1) * 128], wd, wd[:, fc, dh * 512:(dh + 1) * 512],
                         fc == 0, fc == 15)
                k.op("act", "activation", y_[:, dh * 512:(dh + 1) * 512], p_[:, :512], AF.Copy, scale=gval_all[:, ex, st:st + 1],
                     r=[p_, gval_all], w=[y_])

            def fn(e, y_=y_, st=st):
                return e.indirect_dma_start(out=ypart.t[:, :],
                                            out_offset=bass.IndirectOffsetOnAxis(ap=idxs_all[:, ex, st:st + 1], axis=0),
                                            in_=y_[:, :], in_offset=None, compute_op=ALU.add, oob_is_err=True)
            k.S.dma2("pool", fn, y_.b, ypart.b, y_.b)
            idxs_all.b.rd.append(ypart.b.w)

    def load_gu(e):
        wgv = io["w_gate4"].t[l, e].rearrange("(k p) n -> p k n", p=128)
        wuv = io["w_up4"].t[l, e].rearrange("(k p) n -> p k n", p=128)
        for kk in range(8):
            k.dma("pool", wg[:, kk, :], wgv[:, kk, :], None, wg, wg)
            k.dma("pool", wu[:, kk, :], wuv[:, kk, :], None, wu, wu)

    def load_d(e):
        wdv = io["w_down4"].t[l, e].rearrange("(k p) n -> p k n", p=128)
        for kk in range(16):
            k.dma("pool", wd[:, kk, :], wdv[:, kk, :], None, wd, wd)

    load_gu(0)
    load_d(0)
    nst_all = 9 if with_ctx else 8
    for e in range(NE):
        compact(A[:, e, :], pj, e, 64, 8, 1024, 0)
        finish_sel(e, 0, 8, (2048, 16, 512), (512, 16, TT), False)
        if with_ctx:
            compact(Ac[:, e, :], pjc, 4 + e, 2, 1, 32, 8)
            finish_sel(e, 8, 1, (2, 0, 0), (2, 0, 0), True)
    for e in range(NE):
        gather_T(e, 0, nst_all)
        gu(nst_all)
        if e + 1 < NE:
            load_gu(e + 1)
        down(e, nst_all)
        if e + 1 < NE:
            load_d(e + 1)
    k.end_phase()


def emit_d(k, io, l, last, o_xT):
    c = _common(k, io, need_id=True)
    ones, idf = c["ones"], c["idf"]
    ysum, xmidT = io["s_ysum"], io["s_xmidT"]
    fg_s = k.sb("fg_s", [128, 8], F32)
    yt = [k.sb("yt%d" % i, [128, D], F32) for i in range(4)]
    xm2 = [k.sb("xm%d" % i, [128, 8, 512], F32) for i in range(2)]
    xo2 = [k.sb("xo%d" % i, [128, 8, 512], F32) for i in range(2)]
    sq = k.sb("sq", [128, 512], F32)
    rstd = k.sb("rstd", [128, 512], F32)
    lntmp = k.sb("lntmp", [128, 512], F32)
    xn = k.sb("xn", [128, 512], F32)
    pss = k.ps("pss")
    ptr = [k.ps("ptr0", [128, D], F32), k.ps("ptr1", [128, D], F32)]
    k.dma("sp", fg_s[:], io["fgT"][:, :], None, fg_s, fg_s)
    mod = _mod_setup(k, io, l, 40, 8)
    si = [0]
    tiles = TILES[:4] if last else TILES
    xv = xmidT.t.rearrange("(k p) n -> p k n", p=128)
    for ti, (c0, ncs) in enumerate(tiles):
        mi = 1 if ti == 4 else 0
        xm = xm2[ti % 2]
        xo = xo2[ti % 2]
        for kk in range(8):
            k.dma("sp", xm[:, kk, :ncs], xv[:, kk, c0:c0 + ncs], None, xm, xm)
        for tq in range(ncs // 128):
            r0 = c0 + tq * 128
            y_ = yt[si[0] % 4]
            p_ = ptr[si[0] % 2]
            si[0] += 1
            k.dma("act", y_[:, :], ysum[r0:r0 + 128, :], None, y_, y_)
            for kk in range(8):
                k.op("pe", "transpose", p_[:, kk * 128:(kk + 1) * 128], y_[:, kk * 128:(kk + 1) * 128], idf[:, :],
                     r=[y_, idf], w=[p_])
            for kk in range(8):
                k.op("dve", "scalar_tensor_tensor", xo[:, kk, tq * 128:(tq + 1) * 128], p_[:, kk * 128:(kk + 1) * 128],
                     mod[:, kk, mi:mi + 1], xm[:, kk, tq * 128:(tq + 1) * 128], ALU.mult, ALU.add,
                     r=[p_, mod, xm], w=[xo])
        if last:
            for kk in range(8):
                k.op("act", "activation", sq[:, :ncs], xo[:, kk, :ncs], AF.Square, r=[xo], w=[sq])
                k.mm(pss, pss[:, :ncs], ones, ones[:, :], sq, sq[:, :ncs], kk == 0, kk == 7)
            _rsqrt_mean(k, rstd, pss, float(D), ncs, lntmp)
            for kk in range(8):
                k.op("dve", "tensor_tensor", xn[:, :ncs], xo[:, kk, :ncs], rstd[:, :ncs], ALU.mult, r=[xo, rstd], w=[xn])
                k.op("act", "activation", xo[:, kk, :ncs], xn[:, :ncs], AF.Copy, scale=fg_s[:, kk:kk + 1],
                     r=[xn, fg_s], w=[xo])
        k.dma("sp", o_xT.t.rearrange("(k p) n -> p k n", p=128)[:, :, c0:c0 + ncs], xo[:, :, :ncs], xo, o_xT, xo)
    k.end_phase()


def build_fused(dbg=False):
    k = KB()
    io = {}

    def ein(name, shape, dt=F32):
        io[name] = k.din(name, shape, dt)

    ein("xT0", [D, TT]); ein("cT", [128, 8, 2]); ein("w_ada", [2, D, 6 * D]); ein("b_adaT", [2, 128, 48])
    ein("g1T", [2, 128, 8]); ein("g2T", [2, 128, 8]); ein("w_in", [2, D, DIN]); ein("gqT", [2, 128, 3])
    ein("gkvT", [2, 128, 2]); ein("w_uq", [2, 384, 768]); ein("w_ukv", [2, 256, 1024]); ein("ropeT", [32, 2, TL])
    ein("nbias", [2, 4, 128, 8, 384]); ein("pcinv", [128, 2, TT]); ein("w_pool", [2, 4, 64, 64])
    ein("pscaleT", [2, 128, 2]); ein("hmask", [128, 8]); ein("w_br_mla", [2, 512, D]); ein("w_br_na", [2, 256, D])
    ein("w_br_pool", [2, 256, D]); ein("w_out", [2, D, D]); ein("w_router", [2, D, 16]); ein("ident", [128, 128])
    ein("w_gate4", [2, NE, D, FF]); ein("w_up4", [2, NE, D, FF]); ein("w_down4", [2, NE, FF, D])
    ein("c_iota", [128, 1024]); ein("c_pj", [128, 64, 4]); ein("c_pjc", [128, 2, 4]); ein("c_tri", [128, 128])
    ein("c_base", [128, 2]); ein("esel", [128, NE, 16]); ein("fgT", [128, 8])
    io["out"] = k.dout("o_xT", [D, TL], F32)

    def scr(name, shape, dt):
        io[name] = k.dscr(name, shape, dt)

    scr("s_mod", [128, 48, 2], F32)
    scr("s_qT", [768, TT], BF16); scr("s_kT", [544, TT], BF16); scr("s_v", [TT, 512], BF16)
    scr("s_nqT", [256, TT], BF16); scr("s_nkT", [256, TT], BF16); scr("s_nv", [TT, 256], BF16)
    scr("s_poolT", [256, TT], F32); scr("s_gateT", [3072, TT], BF16)
    scr("g_kT", [4 * 544, TT], BF16); scr("g_v", [4 * TT, 512], BF16)
    scr("s_nkh", [256, NHK], BF16); scr("g_nkh", [4 * 256, NHK], BF16)
    scr("s_nvh", [NHK, 256], BF16); scr("g_nvh", [4 * NHK, 256], BF16)
    scr("s_ph", [256, 2 * HALO], F32); scr("g_ph", [4 * 256, 2 * HALO], F32)
    scr("s_kfr", [256, FR * GW], BF16); scr("s_vfr", [FR * GW, 256], BF16)
    scr("s_pfr", [256, TL + 2 * HALO], F32); scr("s_pcf", [256, NCTX + 2 * HALO], F32)
    scr("s_aT", [8, 64, TT], BF16); scr("s_bT", [4, 64, TT], BF16); scr("s_pT", [256, TT], BF16)
    scr("s_xmidT", [D, TT], F32); scr("s_h2", [TT, D], BF16); scr("s_aff", [TT, 16], F32)
    scr("g_h2", [4 * TT, D], BF16); scr("g_aff", [4 * TT, 16], F32)
    scr("s_ypart", [4 * TT, D], F32); scr("s_ysum", [TT, D], F32); scr("s_x1T", [D, TT], F32)

    for l in range(2):
        last = l == 1
        xT = io["xT0"] if l == 0 else io["s_x1T"]
        emit_mod(k, io, l)
        emit_a(k, io, l, xT)
        emit_frames(k, io)
        emit_b1(k, io, l, not last)
        emit_b2(k, io, l, xT, not last)
        k.cc("AllGather", ALU.bypass, io["s_aff"], io["s_aff"][:, :], io["g_aff"], io["g_aff"][:, :])
        k.end_phase()
        emit_c(k, io, l, not last)
        k.cc("ReduceScatter", ALU.add, io["s_ypart"], io["s_ypart"][:, :], io["s_ysum"], io["s_ysum"][:, :])
        k.end_phase()
        emit_d(k, io, l, last, io["out"] if last else io["s_x1T"])
        if dbg and l == 0:
            down = k.sb("down", [128, 1], F32)
            for nm in ("s_aT", "s_bT", "s_pT", "s_xmidT", "s_h2", "s_aff", "s_ysum", "s_x1T", "s_kfr", "s_vfr", "s_pfr"):
                src = io[nm]
                shp = [int(v) for v in src.t.shape]
                dst = k.dout("d_" + nm, shp, src.t.dtype)
                if len(shp) == 3:
                    k.dma("sp", dst[:, :, :], src[:, :, :], src, dst, down)
                else:
                    k.dma("sp", dst[:, :], src[:, :], src, dst, down)
            k.end_phase()
    k.S.finalize()
    return k.nc


def lay128(v):
    v = np.asarray(v)
    return np.ascontiguousarray(v.reshape(-1, 128).T)


def rope_tables(core):
    t0 = (core % 4) * TL
    t = np.arange(t0, t0 + TL)
    pos = [(t // GW).astype(np.float32), (t % GW).astype(np.float32)]
    inv = (np.float32(10000.0) ** (-np.arange(8, dtype=np.float32) / np.float32(8))).astype(np.float32)
    tab = np.zeros((32, 2, TL), np.float32)
    for i in range(32):
        ang = (pos[i // 16] * inv[(i % 16) % 8]).astype(np.float32)
        tab[i, 0] = np.cos(ang)
        tab[i, 1] = np.sin(ang)
    return tab


def na_bias_tables(rpb_all, core):
    rank = core % 4
    r0 = rank * 32
    qc = np.arange(GW)
    start_c = np.clip(qc - 8, 0, GW - 16)
    kcol = np.arange(GW)
    colmask = (kcol[:, None] >= start_c[None, :]) & (kcol[:, None] < start_c[None, :] + 16)
    dc = np.clip(kcol[:, None] - qc[None, :], -15, 15) + 15
    out = np.full((2, 4, 128, 8, 384), -30000.0, np.float32)
    rl_of_var = {0: 10, 1: 0, 2: 1, 3: 2, 4: 3, 5: 29, 6: 30, 7: 31}
    for l in range(2):
        rpb = np.asarray(rpb_all[l])
        for var in range(8):
            rl = rl_of_var[var]
            s0, nch, v_ = na_window(rl)
            assert v_ == var
            g = r0 + rl
            sr = int(np.clip(g - 4, 0, 128 - 8))
            for i in range(nch):
                for hf in range(2):
                    gr = r0 - 7 + s0 + 2 * i + hf
                    if not (sr <= gr < sr + 8):
                        continue
                    dr = gr - g + 7
                    blk = np.where(colmask[None], rpb[:, dr][:, dc], np.float32(-30000.0))
                    out[l, :, hf * 64:(hf + 1) * 64, var, i * 64:(i + 1) * 64] = blk
    return out


def moe_consts():
    c_iota = np.broadcast_to(np.arange(1, 1025, dtype=np.float32)[None, :], (128, 1024)).copy()
    jj = np.arange(64)
    pp = np.arange(128)
    c_pj = np.zeros((128, 64, 4), np.float32)
    c_pj[:, :, 0] = (pp // 32).astype(np.float32)[:, None]
    c_pj[:, :, 1] = (pp % 32).astype(np.float32)[:, None]
    c_pj[:, :, 2] = (jj // 16).astype(np.float32)[None, :]
    c_pj[:, :, 3] = (jj % 16).astype(np.float32)[None, :]
    c_pjc = np.zeros((128, 2, 4), np.float32)
    c_pjc[:, :, 0] = pp.astype(np.float32)[:, None]
    c_pjc[:, :, 3] = np.arange(2, dtype=np.float32)[None, :]
    c_tri = (pp[:, None] < pp[None, :]).astype(np.float32)
    c_base = np.zeros((128, 2), np.float32)
    c_base[0:32, 0] = 4 * TL
    c_base[32:128, 0] = 4 * TL + NCTX + np.arange(96)
    c_base[0:32, 1] = TL
    c_base[32:128, 1] = TT + TL + np.arange(96)
    return c_iota, c_pj, c_pjc, c_tri, c_base


def host_inputs(inp):
    c_iota, c_pj, c_pjc, c_tri, c_base = moe_consts()
    ident = np.eye(128, dtype=np.float32)
    st2 = lambda key: np.ascontiguousarray(np.stack([lay128(inp[key][l]) for l in range(2)], axis=0))
    shared = {
        "w_ada": inp["w_ada"], "b_adaT": st2("b_ada"), "g1T": st2("norm1_g"), "g2T": st2("norm2_g"),
        "w_in": inp["w_in"], "gqT": st2("mla_q_norm"), "gkvT": st2("mla_kv_norm"), "w_uq": inp["w_uq"],
        "w_ukv": inp["w_ukv"], "w_pool": inp["w_pool"], "pscaleT": st2("pool_scale"),
        "w_br_mla": inp["w_br_mla"], "w_br_na": inp["w_br_na"], "w_br_pool": inp["w_br_pool"],
        "w_out": inp["w_out"], "w_router": inp["w_router"], "ident": ident,
        "c_iota": c_iota, "c_pj": c_pj, "c_pjc": c_pjc, "c_tri": c_tri, "c_base": c_base,
        "fgT": lay128(inp["final_g"]),
    }
    maps = []
    for core in range(NCORE):
        b, rank = core // 4, core % 4
        t0 = rank * TL
        m = dict(shared)
        m["xT0"] = np.ascontiguousarray(np.concatenate([inp["x"][b, t0:t0 + TL], inp["ctx"][b]], axis=0).T)
        m["cT"] = np.ascontiguousarray(np.stack([lay128(inp["c"][b]), lay128(inp["c_ctx"])], axis=-1))
        m["ropeT"] = rope_tables(core)
        m["nbias"] = na_bias_tables(inp["na_rpb"], core)
        pcinv = np.zeros((128, 2, TT), np.float32)
        for g, w in enumerate((2, 4, 8, 16)):
            c_, hf = g // 2, g % 2
            t = np.arange(t0, t0 + TL)
            cnt = np.clip(t + w // 2, 0, NSEQ) - np.clip(t - w // 2, 0, NSEQ)
            tcx = np.arange(NCTX)
            cntc = np.clip(tcx + w // 2, 0, NCTX) - np.clip(tcx - w // 2, 0, NCTX)
            pcinv[hf * 64:(hf + 1) * 64, c_, :TL] = (np.float32(1.0) / cnt.astype(np.float32))[None, :]
            pcinv[hf * 64:(hf + 1) * 64, c_, TL:] = (np.float32(1.0) / cntc.astype(np.float32))[None, :]
        m["pcinv"] = pcinv
        hm = np.zeros((128, 8), np.float32)
        if rank > 0:
            hm[:, rank - 1] = 1.0
        if rank < 3:
            hm[:, 4 + rank + 1] = 1.0
        m["hmask"] = hm
        es = np.zeros((128, NE, 16), np.float32)
        for i in range(NE):
            es[:, i, NE * rank + i] = 1.0
        m["esel"] = es
        m["w_gate4"] = np.ascontiguousarray(inp["w_gate"][:, NE * rank:NE * rank + NE])
        m["w_up4"] = np.ascontiguousarray(inp["w_up"][:, NE * rank:NE * rank + NE])
        m["w_down4"] = np.ascontiguousarray(inp["w_down"][:, NE * rank:NE * rank + NE])
        maps.append(m)
    return maps


_NC = []
_DBG = [False, None]


def kernel(**inputs):
    inp = {k_: np.asarray(v) for k_, v in inputs.items()}
    if not _NC:
        _NC.append(build_fused(_DBG[0]))
    res = run_bass_kernel_spmd(_NC[0], host_inputs(inp), core_ids=list(range(NCORE)))
    if _DBG[0]:
        _DBG[1] = res.results
    out = np.empty((NB, NSEQ, D), np.float32)
    for core in range(NCORE):
        b, rank = core // 4, core % 4
        out[b, rank * TL:(rank + 1) * TL] = np.asarray(res.results[core]["o_xT"]).T
    return out
```

```python
import numpy as np
import concourse.bass as bass
import concourse.mybir as mybir
from concourse.bass_utils import run_bass_kernel_spmd
from contextlib import ExitStack

F32 = mybir.dt.float32
BF16 = mybir.dt.bfloat16
I32 = mybir.dt.int32
U32 = mybir.dt.uint32
ALU = mybir.AluOpType
AF = mybir.ActivationFunctionType
AX = mybir.AxisListType


class Buf:
    __slots__ = ("name", "w", "rd", "slot", "slot_phase")

    def __init__(self, name):
        self.name = name
        self.w = None
        self.rd = []
        self.slot = None
        self.slot_phase = -1


class Slot:
    __slots__ = ("sem", "total", "busy")

    def __init__(self):
        self.sem = None
        self.total = 0
        self.busy = False


class Sched:
    ENGS = ("pe", "dve", "act", "pool", "sp")

    def __init__(self, nc):
        self.nc = nc
        self.ins = {e: [] for e in self.ENGS}
        self.sig = {e: [] for e in self.ENGS}
        self.bufs = []
        self.slots = []
        self.phase = 0
        self.pending = {}
        self.last_compute = {e: None for e in self.ENGS}

    def _slot_for(self, buf):
        if buf.slot is None or buf.slot_phase != self.phase:
            s = None
            for c in self.slots:
                if not c.busy:
                    s = c
                    break
            if s is None:
                s = Slot()
                self.slots.append(s)
            s.busy = True
            buf.slot = s
            buf.slot_phase = self.phase
        return buf.slot

    def fence(self, eng):
        evs = [("d", s, s.total) for s in self.slots if s.total > 0]
        self.pending[eng] = self.pending.get(eng, []) + evs

    def barrier(self):
        evs = []
        for e in self.ENGS:
            if self.last_compute[e] is not None:
                evs.append(("e", e, self.last_compute[e]))
        for s in self.slots:
            if s.total > 0:
                evs.append(("d", s, s.total))
            s.busy = False
        self.pending = {e: list(evs) for e in self.ENGS}
        self.phase += 1

    def buf(self, name):
        b = Buf(name)
        self.bufs.append(b)
        return b

    def _add(self, eng, fn, reads, writes, dma_dst=None, inc=16):
        waits = self.pending.pop(eng, [])
        for b in reads:
            if b.w is not None:
                waits.append(b.w)
        for b in writes:
            if b.w is not None and not (b.w[0] == "e" and b.w[1] == eng):
                waits.append(b.w)
            for ev in b.rd:
                if not (ev[0] == "e" and ev[1] == eng):
                    waits.append(ev)
        idx = len(self.ins[eng])
        if dma_dst is not None:
            slot = self._slot_for(dma_dst)
            slot.total += inc
            ev = ("d", slot, slot.total)
            dma_dst = slot
        else:
            ev = ("e", eng, idx)
            self.last_compute[eng] = idx
        self.ins[eng].append([fn, waits, dma_dst, inc])
        self.sig[eng].append(False)
        for b in reads:
            b.rd.append(ev)
        for b in writes:
            b.w = ev
            b.rd = []
        return ev

    def op(self, eng, fn, reads=(), writes=()):
        return self._add(eng, fn, list(reads), list(writes))

    def dma(self, eng, fn, src, dst):
        return self._add(eng, fn, [src] if src is not None else [], [dst], dma_dst=dst)

    def finalize(self, final_bufs=()):
        nc = self.nc
        fin = []
        for b in final_bufs:
            if b.w is not None:
                fin.append(b.w)
        for s in self.slots:
            if s.total > 0:
                fin.append(("d", s, s.total))
        fin = self.pending.pop("sp", []) + fin
        self.ins["sp"].append([None, fin, None, 16])
        self.sig["sp"].append(False)
        for e in self.ENGS:
            for rec in self.ins[e]:
                pruned = []
                for ev in rec[1]:
                    if ev[0] == "e":
                        if ev[1] == e and e == "pe":
                            continue
                        self.sig[ev[1]][ev[2]] = True
                    pruned.append(ev)
                rec[1] = pruned
        cnt_at = {}
        for e in self.ENGS:
            c = 0
            arr = []
            for s in self.sig[e]:
                if s:
                    c += 1
                arr.append(c)
            cnt_at[e] = arr
        sems = {e: nc.alloc_semaphore("s_" + e) for e in self.ENGS}
        for i, s in enumerate(self.slots):
            s.sem = nc.alloc_semaphore("d_%d" % i)
        engh = {"pe": "tensor", "dve": "vector", "act": "scalar", "pool": "gpsimd", "sp": "sync"}
        with nc.Block() as block:
            for e in self.ENGS:
                def body(engine, e=e):
                    seen = {}
                    for i, (fn, waits, dmabuf, inc) in enumerate(self.ins[e]):
                        need = {}
                        for ev in waits:
                            if ev[0] == "e":
                                key = ("e", ev[1])
                                val = cnt_at[ev[1]][ev[2]]
                                sem = sems[ev[1]]
                            else:
                                key = ("d", id(ev[1]))
                                val = ev[2]
                                sem = ev[1].sem
                            if seen.get(key, 0) >= val:
                                continue
                            if key not in need or need[key][1] < val:
                                need[key] = (sem, val)
                        for key, (sem, val) in need.items():
                            engine.wait_ge(sem, val)
                            seen[key] = val
                        if fn is None:
                            continue
                        ins = fn(engine)
                        if dmabuf is not None:
                            ins.then_inc(dmabuf.sem, inc)
                        elif self.sig[e][i]:
                            ins.then_inc(sems[e], 1)
                getattr(block, engh[e])(body)


class T:
    __slots__ = ("t", "b")

    def __init__(self, t, b):
        self.t = t
        self.b = b

    def __getitem__(self, k):
        return self.t[k]


class KB:
    def __init__(self):
        self.nc = bass.Bass("TRN2", target_bir_lowering=False)
        self.S = Sched(self.nc)
        self.pid = 0
        self.stack = ExitStack()

    def end_phase(self):
        self.S.barrier()
        self.stack.close()
        self.stack = ExitStack()
        self.pid += 1

    def sb(self, name, shape, dt):
        nm = "%s_%d" % (name, self.pid)
        return T(self.stack.enter_context(self.nc.sbuf_tensor(nm, list(shape), dt)), self.S.buf(nm))

    def ps(self, name, shape=(128, 512), dt=F32):
        nm = "%s_%d" % (name, self.pid)
        return T(self.stack.enter_context(self.nc.psum_tensor(nm, list(shape), dt)), self.S.buf(nm))

    def dscr(self, name, shape, dt):
        return T(self.nc.dram_tensor(name, list(shape), dt).ap(), self.S.buf(name))

    def cc(self, kind, op, src, src_ap, dst, dst_ap):
        def fn(e):
            return e.collective_compute(kind, op, replica_groups=[[0, 1, 2, 3], [4, 5, 6, 7]],
                                        ins=[src_ap], outs=[dst_ap])
        return self.S.dma2("pool", fn, src.b, dst.b, dst.b, inc=1)

    def din(self, name, shape, dt):
        return T(self.nc.dram_tensor(name, list(shape), dt, kind="ExternalInput").ap(), self.S.buf(name))

    def dout(self, name, shape, dt):
        return T(self.nc.dram_tensor(name, list(shape), dt, kind="ExternalOutput").ap(), self.S.buf(name))

    def op(self, eng, meth, *args, r=(), w=(), **kw):
        def fn(e, meth=meth, args=args, kw=kw):
            return getattr(e, meth)(*args, **kw)
        return self.S.op(eng, fn, [x.b for x in r], [x.b for x in w])

    def dma(self, eng, out_ap, in_ap, src, dst, owner, **kw):
        def fn(e, out_ap=out_ap, in_ap=in_ap, kw=kw):
            return e.dma_start(out=out_ap, in_=in_ap, **kw)
        return self.S.dma2(eng, fn, src.b if src is not None else None, dst.b if dst is not None else None, owner.b)

    def mm(self, out_t, out_ap, lt, lhsT_ap, rt, rhs_ap, start, stop):
        return self.op("pe", "matmul", out_ap, lhsT_ap, rhs_ap, start=start, stop=stop,
                       r=[lt, rt], w=[out_t])


def _dma2(self, eng, fn, src, dst, owner, inc=16):
    reads = [src] if src is not None else []
    writes = [dst] if dst is not None else []
    if owner not in writes and owner not in reads:
        writes = writes + [owner]
    return self._add(eng, fn, reads, writes, dma_dst=owner, inc=inc)


Sched.dma2 = _dma2


D = 1024
NB = 2
NSEQ = 8192
NCTX = 256
GW = 64
NCORE = 8
TL = 2048
TT = TL + NCTX
DIN = 4768
O_CQ, O_CKV, O_KR, O_NA, O_POOL, O_GATE = 0, 384, 640, 672, 1440, 1696
EPS = 1e-6
TILES = [(0, 512), (512, 512), (1024, 512), (1536, 512), (2048, 256)]


def _rsqrt_mean(k, out_t, ss_ps, n, ncols, tmp_t):
    k.op("act", "activation", tmp_t[:, :ncols], ss_ps[:, :ncols], AF.Ln, bias=k.eps_t[:, 0:1], scale=1.0 / n,
         r=[ss_ps, k.eps_t], w=[tmp_t])
    k.op("act", "activation", out_t[:, :ncols], tmp_t[:, :ncols], AF.Exp, scale=-0.5, r=[tmp_t], w=[out_t])


def _mod_vectors(k, w_ada_l, sil, wada_s, pmod, ch0, nch):
    w_ada_v = w_ada_l.rearrange("(k p) n -> p k n", p=128)
    for bi, blk in enumerate(range(ch0 // 4, (ch0 + nch) // 4)):
        wt = wada_s[bi % 2]
        for kk in range(8):
            k.dma("sp", wt[:, kk, :], w_ada_v[:, kk, blk * 512:(blk + 1) * 512], None, wt, wt)
        for j in range(4):
            ci = bi * 4 + j
            for kk in range(8):
                k.mm(pmod, pmod[:, 2 * ci:2 * ci + 2], wt, wt[:, kk, j * 128:(j + 1) * 128], sil, sil[:, kk, :],
                     kk == 0, kk == 7)


def _common(k, io, need_id=False):
    c = {}
    c["ones"] = k.sb("ones", [128, 128], F32)
    k.eps_t = k.sb("eps", [128, 1], F32)
    k.op("dve", "memset", c["ones"][:], 1.0, w=[c["ones"]])
    k.op("dve", "memset", k.eps_t[:], EPS, w=[k.eps_t])
    if need_id:
        c["idf"] = k.sb("idf", [128, 128], F32)
        c["idb"] = k.sb("idb", [128, 128], BF16)
        k.dma("sp", c["idf"][:], io["ident"][:, :], None, c["idf"], c["idf"])
        k.op("dve", "tensor_copy", c["idb"][:], c["idf"][:], r=[c["idf"]], w=[c["idb"]])
    return c


def emit_mod(k, io, l):
    cT_s = k.sb("cT_s", [128, 8, 2], F32)
    sil = k.sb("sil", [128, 8, 2], F32)
    wada_s = [k.sb("wada0", [128, 8, 512], F32), k.sb("wada1", [128, 8, 512], F32)]
    bada_s = k.sb("bada_s", [128, 48], F32)
    mod = k.sb("modall", [128, 48, 2], F32)
    pmod = k.ps("pmod")
    k.dma("sp", cT_s[:], io["cT"][:, :, :], None, cT_s, cT_s)
    k.dma("sp", bada_s[:], io["b_adaT"].t[l], None, bada_s, bada_s)
    k.op("act", "activation", sil[:], cT_s[:], AF.Silu, r=[cT_s], w=[sil])
    _mod_vectors(k, io["w_ada"].t[l], sil, wada_s, pmod, 0, 48)
    k.op("dve", "tensor_tensor", mod[:], pmod[:, 0:96].rearrange("p (c t) -> p c t", t=2),
         bada_s[:, 0:48].unsqueeze(2).to_broadcast([128, 48, 2]), ALU.add, r=[pmod, bada_s], w=[mod])
    k.dma("sp", io["s_mod"][:, :, :], mod[:], mod, io["s_mod"], mod)
    k.end_phase()


def _mod_setup(k, io, l, ch0, nch):
    mod = k.sb("mod", [128, nch, 2], F32)
    k.dma("sp", mod[:], io["s_mod"][:, ch0:ch0 + nch, :], None, mod, mod)
    return mod


def emit_a(k, io, l, xT):
    c = _common(k, io)
    ones = c["ones"]
    o_qT, o_kT, o_v, o_nqT, o_nkT, o_nv, o_poolT, o_gateT = (io[n] for n in
        ("s_qT", "s_kT", "s_v", "s_nqT", "s_nkT", "s_nv", "s_poolT", "s_gateT"))
    win = k.sb("win", [128, 8, DIN], BF16)
    wkr_rot = k.sb("wkr_rot", [128, 8, 32], BF16)
    wuq = k.sb("wuq", [128, 3, 768], BF16)
    wuq_rot = k.sb("wuq_rot", [128, 3, 768], BF16)
    wukv = k.sb("wukv", [128, 2, 1024], BF16)
    wkn = k.sb("wkn", [128, 2, 512], BF16)
    wvv = k.sb("wvv", [128, 2, 512], BF16)
    cs = k.sb("cs", [128, 2, TL], F32)
    g1_s = k.sb("g1_s", [128, 8], F32)
    gq_s = k.sb("gq_s", [128, 3], F32)
    gkv_s = k.sb("gkv_s", [128, 2], F32)
    Acoef = k.sb("Acoef", [128, 8, 2], F32)
    xt = k.sb("xt", [128, 8, 512], F32)
    sq = k.sb("sq", [128, 512], F32)
    rstd = k.sb("rstd", [128, 512], F32)
    lntmp = k.sb("lntmp", [128, 512], F32)
    xn = k.sb("xn", [128, 512], F32)
    hT = k.sb("hT", [128, 8, TT], BF16)
    cq = k.sb("cq", [128, 3, 512], F32)
    cqn = k.sb("cqn", [128, 3, TT], BF16)
    ckv = k.sb("ckv", [128, 2, 512], F32)
    ckvn = k.sb("ckvn", [128, 2, 512], BF16)
    rt1 = k.sb("rt1", [128, 512], F32)
    rt2 = k.sb("rt2", [128, 512], F32)
    NST = 4
    st_bf = [k.sb("st_bf%d" % i, [128, 512], BF16) for i in range(NST)]
    st_f = [k.sb("st_f%d" % i, [128, 512], F32) for i in range(2)]
    pz = [k.ps("pz%d" % i) for i in range(4)]
    pss = k.ps("pss")
    pB = k.ps("pB")

    k.dma("sp", g1_s[:], io["g1T"].t[l], None, g1_s, g1_s)
    k.dma("sp", gq_s[:], io["gqT"].t[l], None, gq_s, gq_s)
    k.dma("sp", gkv_s[:], io["gkvT"].t[l], None, gkv_s, gkv_s)
    k.dma("sp", cs[0:32, :, :], io["ropeT"][:, :, :], None, cs, cs)
    k.dma("sp", cs[64:96, :, :], io["ropeT"][:, :, :], None, cs, cs)
    w_in_v = io["w_in"].t[l].rearrange("(k p) n -> p k n", p=128)
    for kk in range(8):
        for c0 in range(0, DIN, 2048):
            c1 = min(DIN, c0 + 2048)
            k.dma("pool", win[:, kk, c0:c1], w_in_v[:, kk, c0:c1], None, win, win)
    w_uq_v = io["w_uq"].t[l].rearrange("(k p) n -> p k n", p=128)
    for kk in range(3):
        k.dma("pool", wuq[:, kk, :], w_uq_v[:, kk, :], None, wuq, wuq)
    w_ukv_v = io["w_ukv"].t[l].rearrange("(k p) n -> p k n", p=128)
    for kk in range(2):
        k.dma("pool", wukv[:, kk, :], w_ukv_v[:, kk, :], None, wukv, wukv)
    wk_v = wukv[:, :, :].rearrange("p k (h r) -> p k h r", r=128)
    for kk in range(2):
        k.op("dve", "tensor_copy", wkn[:, kk, :].rearrange("p (h r) -> p h r", r=64), wk_v[:, kk, :, 0:64],
             r=[wukv], w=[wkn])
        k.op("dve", "tensor_copy", wvv[:, kk, :].rearrange("p (h r) -> p h r", r=64), wk_v[:, kk, :, 64:128],
             r=[wukv], w=[wvv])
    k.op("pool", "memset", wuq_rot[:], 0.0, w=[wuq_rot])
    for kk in range(3):
        src = wuq[:, kk, :].rearrange("p (h r) -> p h r", r=96)[:, :, 64:96].rearrange(
            "p h (a s j) -> p h a s j", a=2, s=2)
        dst = wuq_rot[:, kk, :].rearrange("p (h r) -> p h r", r=96)[:, :, 64:96].rearrange(
            "p h (a s j) -> p h a s j", a=2, s=2)
        k.op("dve", "tensor_scalar", dst[:, :, :, 0, :], src[:, :, :, 1, :], -1.0, None, ALU.mult,
             r=[wuq], w=[wuq_rot])
        k.op("dve", "tensor_copy", dst[:, :, :, 1, :], src[:, :, :, 0, :], r=[wuq], w=[wuq_rot])
    srck = win[:, :, O_KR:O_KR + 32].rearrange("p k (a s j) -> p k a s j", a=2, s=2)
    dstk = wkr_rot[:, :, :].rearrange("p k (a s j) -> p k a s j", a=2, s=2)
    k.op("dve", "tensor_scalar", dstk[:, :, :, 0, :], srck[:, :, :, 1, :], -1.0, None, ALU.mult,
         r=[win], w=[wkr_rot])
    k.op("dve", "tensor_copy", dstk[:, :, :, 1, :], srck[:, :, :, 0, :], r=[win], w=[wkr_rot])

    mod = _mod_setup(k, io, l, 0, 16)
    k.op("dve", "tensor_scalar", Acoef[:], mod[:, 8:16, :], 1.0, None, ALU.add, r=[mod], w=[Acoef])
    k.op("dve", "tensor_tensor", Acoef[:], Acoef[:], g1_s[:, :].unsqueeze(2).to_broadcast([128, 8, 2]), ALU.mult,
         r=[Acoef, g1_s], w=[Acoef])

    sti = [0]

    def store_bf(dst_t, dst_ap, src_ps_ap, src_ps, np_, ncols, eng="act", func=None):
        s_ = st_bf[sti[0] % NST]
        sti[0] += 1
        if eng == "act":
            k.op("act", "activation", s_[:np_, :ncols], src_ps_ap, func or AF.Copy, r=[src_ps], w=[s_])
        else:
            k.op("dve", "tensor_copy", s_[:np_, :ncols], src_ps_ap, r=[src_ps], w=[s_])
        k.dma("sp", dst_ap, s_[:np_, :ncols], s_, dst_t, s_)

    zi = [0]

    def nextpz():
        p = pz[zi[0] % 4]
        zi[0] += 1
        return p

    xT_v = xT.t.rearrange("(k p) n -> p k n", p=128)
    def p1a(ti, c0, ncs):
        is_ctx = ti == 4
        mi = 1 if is_ctx else 0
        k.dma("sp", xt[:, :, :ncs], xT_v[:, :, c0:c0 + ncs], None, xt, xt)
        for kk in range(8):
            k.op("act", "activation", sq[:, :ncs], xt[:, kk, :ncs], AF.Square, r=[xt], w=[sq])
            k.mm(pss, pss[:, :ncs], ones, ones[:, :], sq, sq[:, :ncs], kk == 0, kk == 7)
        _rsqrt_mean(k, rstd, pss, float(D), ncs, lntmp)
        for kk in range(8):
            k.op("dve", "tensor_tensor", xn[:, :ncs], xt[:, kk, :ncs], rstd[:, :ncs], ALU.mult, r=[xt, rstd], w=[xn])
            k.op("act", "activation", hT[:, kk, c0:c0 + ncs], xn[:, :ncs], AF.Identity,
                 bias=mod[:, kk, mi:mi + 1], scale=Acoef[:, kk, mi:mi + 1], r=[xn, mod, Acoef], w=[hT])


    def p1b(ti, c0, ncs):
        is_ctx = ti == 4
        def zchunk(col0, m):
            p = nextpz()
            for kk in range(8):
                k.mm(p, p[:m, :ncs], win, win[:, kk, col0:col0 + m], hT, hT[:, kk, c0:c0 + ncs], kk == 0, kk == 7)
            return p

        for (off, nch, raw, nrm, gs, n, nc0) in ((O_CQ, 3, cq, cqn, gq_s, 384.0, c0), (O_CKV, 2, ckv, ckvn, gkv_s, 256.0, 0)):
            for j in range(nch):
                p = zchunk(off + j * 128, 128)
                k.op("dve", "tensor_copy", raw[:, j, :ncs], p[:, :ncs], r=[p], w=[raw])
            for j in range(nch):
                k.op("act", "activation", sq[:, :ncs], raw[:, j, :ncs], AF.Square, r=[raw], w=[sq])
                k.mm(pss, pss[:, :ncs], ones, ones[:, :], sq, sq[:, :ncs], j == 0, j == nch - 1)
            _rsqrt_mean(k, rstd, pss, n, ncs, lntmp)
            for j in range(nch):
                k.op("dve", "tensor_tensor", xn[:, :ncs], raw[:, j, :ncs], rstd[:, :ncs], ALU.mult, r=[raw, rstd], w=[xn])
                k.op("act", "activation", nrm[:, j, nc0:nc0 + ncs], xn[:, :ncs], AF.Copy, scale=gs[:, j:j + 1],
                     r=[xn, gs], w=[nrm])

        p = nextpz()
        for kk in range(8):
            k.mm(p, p[:32, :ncs], win, win[:, kk, O_KR:O_KR + 32], hT, hT[:, kk, c0:c0 + ncs], kk == 0, kk == 7)
        if not is_ctx:
            for kk in range(8):
                k.mm(pB, pB[:32, :ncs], wkr_rot, wkr_rot[:, kk, :], hT, hT[:, kk, c0:c0 + ncs], kk == 0, kk == 7)
            k.op("dve", "tensor_tensor", rt1[0:32, :ncs], p[0:32, :ncs], cs[0:32, 0, c0:c0 + ncs], ALU.mult,
                 r=[p, cs], w=[rt1])
            k.op("dve", "tensor_tensor", rt2[0:32, :ncs], pB[0:32, :ncs], cs[0:32, 1, c0:c0 + ncs], ALU.mult,
                 r=[pB, cs], w=[rt2])
            s_ = st_bf[sti[0] % NST]
            sti[0] += 1
            k.op("dve", "tensor_tensor", s_[0:32, :ncs], rt1[0:32, :ncs], rt2[0:32, :ncs], ALU.add,
                 r=[rt1, rt2], w=[s_])
            k.dma("sp", o_kT[512:544, c0:c0 + ncs], s_[0:32, :ncs], s_, o_kT, s_)
        else:
            store_bf(o_kT, o_kT[512:544, c0:c0 + ncs], p[0:32, :ncs], p, 32, ncs)

        for j in range(2):
            p = zchunk(O_NA + 256 + j * 128, 128)
            store_bf(o_nkT, o_nkT[j * 128:(j + 1) * 128, c0:c0 + ncs], p[:, :ncs], p, 128, ncs, eng="dve")
        for tq in range(ncs // 128):
            p = nextpz()
            for kk in range(8):
                k.mm(p, p[:, :256], hT, hT[:, kk, c0 + tq * 128:c0 + (tq + 1) * 128], win, win[:, kk, O_NA + 512:O_NA + 768],
                     kk == 0, kk == 7)
            store_bf(o_nv, o_nv[c0 + tq * 128:c0 + (tq + 1) * 128, :], p[:, :256], p, 128, 256, eng="dve")

        for j in range(2):
            p = zchunk(O_POOL + j * 128, 128)
            s_ = st_f[j]
            k.op("dve", "tensor_copy", s_[:, :ncs], p[:, :ncs], r=[p], w=[s_])
            k.dma("sp", o_poolT[j * 128:(j + 1) * 128, c0:c0 + ncs], s_[:, :ncs], s_, o_poolT, s_)

        for hp in range(4):
            p = nextpz()
            for kk in range(2):
                k.mm(p, p[:, :ncs], wkn, wkn[:, kk, hp * 128:(hp + 1) * 128], ckvn, ckvn[:, kk, :ncs], kk == 0, kk == 1)
            store_bf(o_kT, o_kT[hp * 128:(hp + 1) * 128, c0:c0 + ncs], p[:, :ncs], p, 128, ncs, eng="dve")
        for tq in range(ncs // 128):
            p = nextpz()
            for kk in range(2):
                k.mm(p, p[:, :512], ckvn, ckvn[:, kk, tq * 128:(tq + 1) * 128], wvv, wvv[:, kk, :],
                     kk == 0, kk == 1)
            store_bf(o_v, o_v[c0 + tq * 128:c0 + (tq + 1) * 128, :], p[:, :512], p, 128, 512)


    def p2(ti, c0, ncs):
        is_ctx = ti == 4
        def zchunk(col0, m):
            p = nextpz()
            for kk in range(8):
                k.mm(p, p[:m, :ncs], win, win[:, kk, col0:col0 + m], hT, hT[:, kk, c0:c0 + ncs], kk == 0, kk == 7)
            return p

        for j in range(2):
            p = zchunk(O_NA + j * 128, 128)
            store_bf(o_nqT, o_nqT[j * 128:(j + 1) * 128, c0:c0 + ncs], p[:, :ncs], p, 128, ncs)
        for j in range(24):
            p = zchunk(O_GATE + j * 128, 128)
            store_bf(o_gateT, o_gateT[j * 128:(j + 1) * 128, c0:c0 + ncs], p[:, :ncs], p, 128, ncs, func=AF.Sigmoid)

        for h in range(8):
            p = nextpz()
            for kk in range(3):
                k.mm(p, p[:96, :ncs], wuq, wuq[:, kk, h * 96:(h + 1) * 96], cqn, cqn[:, kk, c0:c0 + ncs], kk == 0, kk == 2)
            s_ = st_bf[sti[0] % NST]
            sti[0] += 1
            if not is_ctx:
                for kk in range(3):
                    k.mm(pB, pB[:96, :ncs], wuq_rot, wuq_rot[:, kk, h * 96:(h + 1) * 96], cqn, cqn[:, kk, c0:c0 + ncs],
                         kk == 0, kk == 2)
                k.op("act", "activation", s_[0:64, :ncs], p[0:64, :ncs], AF.Copy, r=[p], w=[s_])
                k.op("dve", "tensor_tensor", rt1[64:96, :ncs], p[64:96, :ncs], cs[64:96, 0, c0:c0 + ncs], ALU.mult,
                     r=[p, cs], w=[rt1])
                k.op("dve", "tensor_tensor", rt2[64:96, :ncs], pB[64:96, :ncs], cs[64:96, 1, c0:c0 + ncs], ALU.mult,
                     r=[pB, cs], w=[rt2])
                k.op("dve", "tensor_tensor", s_[64:96, :ncs], rt1[64:96, :ncs], rt2[64:96, :ncs], ALU.add,
                     r=[rt1, rt2], w=[s_])
            else:
                k.op("act", "activation", s_[0:96, :ncs], p[0:96, :ncs], AF.Copy, r=[p], w=[s_])
            k.dma("sp", o_qT[h * 96:(h + 1) * 96, c0:c0 + ncs], s_[0:96, :ncs], s_, o_qT, s_)


    p1a(0, *TILES[0])
    for t in range(5):
        if t + 1 < 5:
            p1a(t + 1, *TILES[t + 1])
        p1b(t, *TILES[t])
        if t >= 2:
            p2(t - 2, *TILES[t - 2])

    k.S.fence("sp")
    cown = k.sb("cown", [128, 1], F32)
    nkT, nv, poolT = io["s_nkT"], io["s_nv"], io["s_poolT"]
    nkh, nvh, ph = io["s_nkh"], io["s_nvh"], io["s_ph"]
    k.dma("sp", nkh[:, 0:7 * GW], nkT[:, TL - 7 * GW:TL], nkT, nkh, cown)
    k.dma("sp", nkh[:, 7 * GW:15 * GW], nkT[:, 0:8 * GW], nkT, nkh, cown)
    k.dma("sp", nkh[:, 15 * GW:NHK], nkT[:, TL:TT], nkT, nkh, cown)
    k.dma("sp", nvh[0:7 * GW, :], nv[TL - 7 * GW:TL, :], nv, nvh, cown)
    k.dma("sp", nvh[7 * GW:15 * GW, :], nv[0:8 * GW, :], nv, nvh, cown)
    k.dma("sp", nvh[15 * GW:NHK, :], nv[TL:TT, :], nv, nvh, cown)
    k.dma("sp", ph[:, 0:HALO], poolT[:, TL - HALO:TL], poolT, ph, cown)
    k.dma("sp", ph[:, HALO:2 * HALO], poolT[:, 0:HALO], poolT, ph, cown)
    k.S.fence("pool")
    for off, cn in KT_CH:
        k.cc("AllGather", ALU.bypass, io["s_kT"], io["s_kT"][off:off + cn, :], io["g_kT"],
             io["g_kT"][4 * off:4 * off + 4 * cn, :])
    for c3 in range(3):
        k.cc("AllGather", ALU.bypass, io["s_v"], io["s_v"][c3 * V_CH:(c3 + 1) * V_CH, :], io["g_v"],
             io["g_v"][c3 * 4 * V_CH:(c3 + 1) * 4 * V_CH, :])
    for s_, g_ in (("s_nkh", "g_nkh"), ("s_nvh", "g_nvh"), ("s_ph", "g_ph")):
        k.cc("AllGather", ALU.bypass, io[s_], io[s_][:, :], io[g_], io[g_][:, :])

    p2(3, *TILES[3])
    p2(4, *TILES[4])
    k.end_phase()


NKEY = NCTX + NSEQ
NKC = NKEY // 128
MLA_SCALE = float(96 ** -0.5)
NA_SCALE = float(64 ** -0.5)
HALO = 8
FR = 47


KT_CH = [(0, 192), (192, 192), (384, 160)]
V_CH = 768
H2_CH = [(0, 512), (512, 512), (1024, 512), (1536, 512), (2048, 256)]
NHK = 7 * GW + 8 * GW + NCTX


def kt_rows(q, i0, n):
    for off, cn in KT_CH:
        if off <= i0 and i0 + n <= off + cn:
            r = 4 * off + q * cn + (i0 - off)
            return slice(r, r + n)
    raise AssertionError


def v_rows(q, t0, n):
    c = t0 // V_CH
    assert (t0 + n - 1) // V_CH == c
    r = c * 4 * V_CH + q * V_CH + (t0 - c * V_CH)
    return slice(r, r + n)


def na_window(rl):
    if rl < 4:
        return rl + 3, 6, 1 + rl
    if rl >= 29:
        return rl - 1, 6, 5 + (rl - 29)
    return rl + 3, 4, 0


def emit_frames(k, io):
    hm = k.sb("hm", [128, 8], F32)
    k.dma("sp", hm[:], io["hmask"][:, :], None, hm, hm)
    kfr, vfr, pfr = io["s_kfr"], io["s_vfr"], io["s_pfr"]
    nkT, nv, poolT = io["s_nkT"], io["s_nv"], io["s_poolT"]
    own = k.sb("own_dummy", [128, 1], F32)
    k.dma("sp", kfr[:, 7 * GW:39 * GW], nkT[:, 0:TL], nkT, kfr, own)
    k.dma("sp", vfr[7 * GW:39 * GW, :], nv[0:TL, :], nv, vfr, own)
    k.dma("sp", pfr[:, HALO:HALO + TL], poolT[:, 0:TL], poolT, pfr, own)

    def select(cand, acc, np_, base):
        k.op("dve", "tensor_scalar", acc[:], cand[:, 0], hm[:np_, base:base + 1], None, ALU.mult, r=[cand, hm], w=[acc])
        for q in range(1, 4):
            k.op("dve", "scalar_tensor_tensor", acc[:], cand[:, q], hm[:np_, base + q:base + q + 1], acc[:],
                 ALU.mult, ALU.add, r=[cand, hm, acc], w=[acc])

    nkh_g, nvh_g, ph_g = io["g_nkh"], io["g_nvh"], io["g_ph"]
    for (nm, ntok, c_lo, f_lo, base) in (("kp", 7 * GW, 0, 0, 0), ("kn", 8 * GW, 7 * GW, 39 * GW, 4)):
        cand = k.sb("cand_" + nm, [128, 4, 2, ntok], BF16)
        acc = k.sb("acc_" + nm, [128, 2, ntok], BF16)
        for q in range(4):
            k.dma("sp", cand[:, q, :, :], nkh_g[q * 256:(q + 1) * 256, c_lo:c_lo + ntok].rearrange("(c p) n -> p c n", p=128),
                  nkh_g, cand, cand)
        select(cand, acc, 128, base)
        k.dma("sp", kfr[:, f_lo:f_lo + ntok].rearrange("(c p) n -> p c n", p=128), acc[:], acc, kfr, acc)
    for (nm, nrow, r_lo, f_lo, base) in (("vp", 7, 0, 0, 0), ("vn", 8, 7, 39, 4)):
        cand = k.sb("cand_" + nm, [64, 4, nrow, 256], BF16)
        acc = k.sb("acc_" + nm, [64, nrow, 256], BF16)
        for q in range(4):
            k.dma("sp", cand[:, q, :, :],
                  nvh_g[q * NHK + r_lo * GW:q * NHK + (r_lo + nrow) * GW, :].rearrange("(a p) n -> p a n", p=64),
                  nvh_g, cand, cand)
        select(cand, acc, 64, base)
        k.dma("sp", vfr[f_lo * GW:(f_lo + nrow) * GW, :].rearrange("(a p) n -> p a n", p=64), acc[:], acc, vfr, acc)
    for (nm, c_lo, f_lo, base) in (("pp", 0, 0, 0), ("pn", HALO, HALO + TL, 4)):
        cand = k.sb("cand_" + nm, [128, 4, 2, HALO], F32)
        acc = k.sb("acc_" + nm, [128, 2, HALO], F32)
        for q in range(4):
            k.dma("sp", cand[:, q, :, :], ph_g[q * 256:(q + 1) * 256, c_lo:c_lo + HALO].rearrange("(c p) n -> p c n", p=128),
                  ph_g, cand, cand)
        select(cand, acc, 128, base)
        k.dma("sp", pfr[:, f_lo:f_lo + HALO].rearrange("(c p) n -> p c n", p=128), acc[:], acc, pfr, acc)
    zc = k.sb("zc", [128, 2, HALO], F32)
    k.op("dve", "memset", zc[:], 0.0, w=[zc])
    pcf = io["s_pcf"]
    k.dma("sp", pcf[:, 0:HALO].rearrange("(c p) n -> p c n", p=128), zc[:], zc, pcf, zc)
    k.dma("sp", pcf[:, HALO + NCTX:2 * HALO + NCTX].rearrange("(c p) n -> p c n", p=128), zc[:], zc, pcf, zc)
    k.dma("sp", pcf[:, HALO:HALO + NCTX], poolT[:, TL:TT], poolT, pcf, own)
    k.end_phase()


def emit_b1(k, io, l, with_ctx_q):
    c = _common(k, io)
    ones = c["ones"]
    qT, kT_g, v_g, nqT = io["s_qT"], io["g_kT"], io["g_v"], io["s_nqT"]
    nkh_g, nvh_g = io["g_nkh"], io["g_nvh"]
    kfr, vfr, pfr, pcf = io["s_kfr"], io["s_vfr"], io["s_pfr"], io["s_pcf"]
    o_aT, o_bT, o_pT = io["s_aT"], io["s_bT"], io["s_pT"]

    khT = [k.sb("khT%d" % i, [96, NKEY], BF16) for i in range(2)]
    vh = [k.sb("vh%d" % i, [128, NKC, 65], BF16) for i in range(2)]
    qh = [k.sb("qh%d" % i, [96, TT], BF16) for i in range(2)]
    pT = [k.sb("pT%d" % i, [128, 512], BF16) for i in range(6)]
    rrow = k.sb("rrow", [128, 512], F32)
    bcs = k.sb("bcs", [64, 512], F32)
    ost = [k.sb("ost%d" % i, [64, 512], BF16) for i in range(2)]
    NPS = 4
    ps_s = [k.ps("ps_s%d" % i) for i in range(NPS)]
    ps_o = [k.ps("ps_o%d" % i) for i in range(2)]
    ps_bc = k.ps("ps_bc")
    ps_z = k.ps("ps_z")
    for i in range(2):
        k.op("pool", "memset", vh[i][:, :, 64:65], 1.0, w=[vh[i]])

    oi = [0]

    def normalize_store(po, ncols, dst_t, dst_ap):
        k.op("dve", "reciprocal", rrow[64:65, :ncols], po[64:65, :ncols], r=[po], w=[rrow])
        k.mm(ps_bc, ps_bc[0:64, :ncols], ones, ones[64:65, 0:64], rrow, rrow[64:65, :ncols], True, True)
        k.op("act", "activation", bcs[:, :ncols], ps_bc[0:64, :ncols], AF.Copy, r=[ps_bc], w=[bcs])
        o_ = ost[oi[0] % 2]
        oi[0] += 1
        k.op("dve", "tensor_tensor", o_[:, :ncols], po[0:64, :ncols], bcs[:, :ncols], ALU.mult, r=[po, bcs], w=[o_])
        k.dma("sp", dst_ap, o_[:, :ncols], o_, dst_t, o_)

    wbd = k.sb("wbd", [128, 2, 128], BF16)
    wbd_f = k.sb("wbd_f", [128, 2, 128], F32)
    psc = k.sb("psc", [128, 2], F32)
    k.op("pool", "memset", wbd_f[:], 0.0, w=[wbd_f])
    for g in range(4):
        c_, hf = g // 2, g % 2
        k.dma("sp", wbd_f[hf * 64:(hf + 1) * 64, c_, hf * 64:(hf + 1) * 64], io["w_pool"].t[l, g], None, wbd_f, wbd_f)
    k.op("dve", "tensor_copy", wbd[:], wbd_f[:], r=[wbd_f], w=[wbd])
    k.dma("sp", psc[:], io["pscaleT"].t[l], None, psc, psc)
    LMAX = TL + 2 * HALO
    u = k.sb("pu", [128, 2, LMAX], F32)
    lv = [k.sb("plv0", [128, 2, LMAX], F32), k.sb("plv1", [128, 2, LMAX], F32)]
    cinv = k.sb("cinv", [128, 2, TT], F32)
    dlt = k.sb("dlt", [128, 2, TT], BF16)
    dtmp = k.sb("dtmp", [128, TL], F32)
    pst = [k.sb("pst%d" % i, [128, 512], BF16) for i in range(2)]
    k.dma("sp", cinv[:], io["pcinv"][:, :, :], None, cinv, cinv)
    uc = k.sb("puc", [128, 2, NCTX + 2 * HALO], F32)
    segs = [(pfr, TL, 0, u)]
    if with_ctx_q:
        segs.append((pcf, NCTX, TL, uc))
    for (src, n, col0, u) in segs:
        L = n + 2 * HALO
        k.dma("sp", u[:, :, 0:L], src.t.rearrange("(c p) n -> p c n", p=128), None, u, u)
        for g in range(4):
            c_, hf = g // 2, g % 2
            dst_t = lv[g % 2]
            k.op("pool", "memset", dst_t[:, :, 0:L], 0.0, w=[dst_t])
            if g == 0:
                k.op("pool", "tensor_tensor", dst_t[:, :, 1:L], u[:, :, 0:L - 1], u[:, :, 1:L], ALU.add, r=[u], w=[dst_t])
            else:
                pv = lv[(g - 1) % 2]
                d = (1 << g) // 2
                k.op("pool", "tensor_tensor", dst_t[:, :, d:L - d], pv[:, :, 0:L - 2 * d], pv[:, :, 2 * d:L], ALU.add,
                     r=[pv], w=[dst_t])
            ps_ = slice(hf * 64, (hf + 1) * 64)
            k.op("dve", "tensor_tensor", dtmp[ps_, 0:n], dst_t[ps_, c_, HALO:HALO + n], cinv[ps_, c_, col0:col0 + n],
                 ALU.mult, r=[dst_t, cinv], w=[dtmp])
            k.op("dve", "tensor_tensor", dlt[ps_, c_, col0:col0 + n], dtmp[ps_, 0:n], u[ps_, c_, HALO:HALO + n], ALU.subtract,
                 r=[dtmp, u], w=[dlt])

    si = [0]
    for h in range(8):
        K_ = khT[h % 2]
        V_ = vh[h % 2]
        Q_ = qh[h % 2]
        k.dma("sp", K_[0:64, 0:NCTX], kT_g[kt_rows(0, h * 64, 64), TL:TT], None, K_, K_)
        k.dma("sp", K_[64:96, 0:NCTX], kT_g[kt_rows(0, 512, 32), TL:TT], None, K_, K_)
        k.dma("sp", V_[:, 0:2, 0:64], v_g[v_rows(0, TL, NCTX), h * 64:(h + 1) * 64].rearrange("(c p) n -> p c n", p=128),
              None, V_, V_)
        for q in range(4):
            k.dma("sp", K_[0:64, NCTX + q * TL:NCTX + (q + 1) * TL], kT_g[kt_rows(q, h * 64, 64), 0:TL], None, K_, K_)
            k.dma("sp", K_[64:96, NCTX + q * TL:NCTX + (q + 1) * TL], kT_g[kt_rows(q, 512, 32), 0:TL], None, K_, K_)
            for c3 in range(3):
                t0_ = c3 * V_CH
                n_ = min(V_CH, TL - t0_)
                k.dma("sp", V_[:, 2 + q * 16 + t0_ // 128:2 + q * 16 + (t0_ + n_) // 128, 0:64],
                      v_g[v_rows(q, t0_, n_), h * 64:(h + 1) * 64].rearrange("(c p) n -> p c n", p=128), None, V_, V_)
        k.dma("sp", Q_[:, :], qT[h * 96:(h + 1) * 96, :], None, Q_, Q_)
        qtiles = [(0, 512, NKC), (512, 512, NKC), (1024, 512, NKC), (1536, 512, NKC)]
        if with_ctx_q:
            qtiles.append((2048, 256, 2))
        for (q0, nq, nkc) in qtiles:
            po = ps_o[oi[0] % 2]
            base = si[0]
            si[0] += nkc

            def s_mm(kc, K_=K_, Q_=Q_, q0=q0, nq=nq, base=base):
                s_ = ps_s[(base + kc) % NPS]
                k.mm(s_, s_[:, :nq], K_, K_[0:96, kc * 128:(kc + 1) * 128], Q_, Q_[0:96, q0:q0 + nq], True, True)

            for kc in range(min(NPS - 1, nkc)):
                s_mm(kc)
            for kc in range(nkc):
                if kc + NPS - 1 < nkc:
                    s_mm(kc + NPS - 1)
                s_ = ps_s[(base + kc) % NPS]
                p_ = pT[(base + kc) % 6]
                k.op("act", "activation", p_[:, :nq], s_[:, :nq], AF.Exp, scale=MLA_SCALE, r=[s_], w=[p_])
                k.mm(po, po[0:65, :nq], V_, V_[:, kc, 0:65], p_, p_[:, :nq], kc == 0, kc == nkc - 1)
            normalize_store(po, nq, o_aT, o_aT[h, :, q0:q0 + nq])

    eb = k.sb("eb", [128, 8, 384], F32)
    kf = k.sb("kf", [64, FR * GW], BF16)
    ve = k.sb("ve", [128, 23, 65], BF16)
    vo = k.sb("vo", [128, 23, 65], BF16)
    nkc_s = k.sb("nkc_s", [64, NCTX], BF16)
    nvc_s = k.sb("nvc_s", [128, 2, 65], BF16)
    nq_s = k.sb("nq_s", [64, TT], BF16)
    e32 = [k.sb("e32_%d" % i, [128, 512], F32) for i in range(2)]
    pb = [k.sb("pb%d" % i, [128, 512], BF16) for i in range(2)]
    k.op("pool", "memset", ve[:, :, 64:65], 1.0, w=[ve])
    k.op("pool", "memset", vo[:, :, 64:65], 1.0, w=[vo])
    k.op("pool", "memset", nvc_s[:, :, 64:65], 1.0, w=[nvc_s])
    ri = [0]
    for h in range(4):
        hs = slice(h * 64, (h + 1) * 64)
        k.dma("sp", eb[:, :, :], io["nbias"].t[l, h], None, eb, eb)
        k.op("act", "activation", eb[:], eb[:], AF.Exp, r=[eb], w=[eb])
        k.dma("sp", nkc_s[:, :], nkh_g[hs, 15 * GW:NHK], None, nkc_s, nkc_s)
        k.dma("sp", nvc_s[:, :, 0:64], nvh_g[15 * GW:NHK, hs].rearrange("(c p) n -> p c n", p=128), None, nvc_s, nvc_s)
        k.dma("sp", nq_s[:, :], nqT[hs, :], None, nq_s, nq_s)
        k.dma("sp", kf[:, :], kfr[hs, :], None, kf, kf)
        k.dma("sp", ve[:, :, 0:64], vfr[0:46 * GW, hs].rearrange("(c p) n -> p c n", p=128), None, ve, ve)
        k.dma("sp", vo[:, :, 0:64], vfr[GW:47 * GW, hs].rearrange("(c p) n -> p c n", p=128), None, vo, vo)
        nbase = si[0]
        si[0] += 32

        def na_s(rl, nbase=nbase):
            s0, nch, var = na_window(rl)
            s_ = ps_s[(nbase + rl) % NPS]
            qcols = nq_s[:, rl * 64:(rl + 1) * 64]
            for i in range(nch):
                k.mm(s_, s_[:, i * 64:(i + 1) * 64], kf, kf[:, (s0 + 2 * i) * GW:(s0 + 2 * i + 2) * GW], nq_s, qcols,
                     True, True)
            for c_ in range(2):
                k.mm(s_, s_[:, (nch + c_) * 64:(nch + c_ + 1) * 64], nkc_s, nkc_s[:, c_ * 128:(c_ + 1) * 128], nq_s, qcols,
                     True, True)

        na_s(0)
        na_s(1)
        for rl in range(32):
            rg, r8 = rl // 8, rl % 8
            if r8 == 0:
                po = ps_o[oi[0] % 2]
            if rl + 2 < 32:
                na_s(rl + 2)
            s0, nch, var = na_window(rl)
            s_ = ps_s[(nbase + rl) % NPS]
            nl = nch * 64
            e_ = e32[ri[0] % 2]
            b_ = pb[ri[0] % 2]
            ri[0] += 1
            k.op("act", "activation", e_[:, 0:nl + 128], s_[:, 0:nl + 128], AF.Exp, scale=NA_SCALE, r=[s_], w=[e_])
            k.op("dve", "tensor_tensor", b_[:, 0:nl], e_[:, 0:nl], eb[:, var, 0:nl], ALU.mult, r=[e_, eb], w=[b_])
            k.op("pool", "tensor_copy", b_[:, nl:nl + 128], e_[:, nl:nl + 128], r=[e_], w=[b_])
            vt = ve if s0 % 2 == 0 else vo
            c0_ = s0 // 2
            for i in range(nch):
                k.mm(po, po[0:65, r8 * 64:(r8 + 1) * 64], vt, vt[:, c0_ + i, 0:65], b_, b_[:, i * 64:(i + 1) * 64],
                     i == 0, False)
            for c_ in range(2):
                k.mm(po, po[0:65, r8 * 64:(r8 + 1) * 64], nvc_s, nvc_s[:, c_, 0:65], b_,
                     b_[:, (nch + c_) * 64:(nch + c_ + 1) * 64], False, c_ == 1)
            if r8 == 7:
                normalize_store(po, 512, o_bT, o_bT[h, :, rg * 512:(rg + 1) * 512])
        if with_ctx_q:
            po = ps_o[oi[0] % 2]
            for c_ in range(2):
                s_ = ps_s[si[0] % NPS]
                p_ = pT[si[0] % 6]
                si[0] += 1
                k.mm(s_, s_[:, :256], nkc_s, nkc_s[:, c_ * 128:(c_ + 1) * 128], nq_s, nq_s[:, 2048:2304], True, True)
                k.op("act", "activation", p_[:, :256], s_[:, :256], AF.Exp, scale=NA_SCALE, r=[s_], w=[p_])
                k.mm(po, po[0:65, :256], nvc_s, nvc_s[:, c_, 0:65], p_, p_[:, :256], c_ == 0, c_ == 1)
            normalize_store(po, 256, o_bT, o_bT[h, :, 2048:2304])

    for (src, n, col0, u_) in segs:
        for c_ in range(2):
            for t0 in range(0, n, 512):
                nn = min(512, n - t0)
                k.mm(ps_z, ps_z[:, :nn], wbd, wbd[:, c_, :], dlt, dlt[:, c_, col0 + t0:col0 + t0 + nn], True, True)
                s_ = pst[oi[0] % 2]
                oi[0] += 1
                k.op("act", "activation", s_[:, :nn], ps_z[:, :nn], AF.Copy, scale=psc[:, c_:c_ + 1], r=[ps_z, psc], w=[s_])
                k.dma("sp", o_pT[c_ * 128:(c_ + 1) * 128, col0 + t0:col0 + t0 + nn], s_[:, :nn], s_, o_pT, s_)


    k.end_phase()


def emit_b2(k, io, l, xT, with_ctx):
    c = _common(k, io, need_id=True)
    ones, idb = c["ones"], c["idb"]
    aT, bT, pT, gateT = io["s_aT"], io["s_bT"], io["s_pT"], io["s_gateT"]
    o_xmidT, o_h2, o_aff = io["s_xmidT"], io["s_h2"], io["s_aff"]
    wmla = k.sb("wmla", [64, 8, D], BF16)
    wna = k.sb("wna", [64, 4, D], BF16)
    wpl = k.sb("wpl", [128, 2, D], BF16)
    wout = k.sb("wout", [128, 8, D], BF16)
    wr = k.sb("wr", [128, 8, 16], F32)
    g2_s = k.sb("g2_s", [128, 8], F32)
    Acoef = k.sb("Acoef", [128, 8, 2], F32)
    a_t = k.sb("a_t", [64, 8, 512], BF16)
    b_t = k.sb("b_t", [64, 4, 512], BF16)
    p_t = k.sb("p_t", [128, 2, 512], BF16)
    g_t = k.sb("g_t", [128, 24, 512], BF16)
    xt = k.sb("xt", [128, 8, 512], F32)
    mT = k.sb("mT", [128, 8, 512], BF16)
    xmid = k.sb("xmid", [128, 8, 512], F32)
    h2f = k.sb("h2f", [128, 8, 512], F32)
    h2b = k.sb("h2b", [128, 8, 512], BF16)
    t1 = k.sb("t1", [128, 512], F32)
    t2 = k.sb("t2", [128, 512], F32)
    sq = k.sb("sq", [128, 512], F32)
    rstd = k.sb("rstd", [128, 512], F32)
    lntmp = k.sb("lntmp", [128, 512], F32)
    xn = k.sb("xn", [128, 512], F32)
    htok = [k.sb("htok%d" % i, [128, D], BF16) for i in range(2)]
    afft = [k.sb("afft%d" % i, [128, 16], F32) for i in range(2)]
    sm = k.sb("sm", [128, 8], F32)
    ex = k.sb("ex", [128, 16], F32)
    pa = k.ps("pa")
    pb_ = k.ps("pb")
    pp = k.ps("pp")
    po = [k.ps("po0"), k.ps("po1")]
    pss = k.ps("pss")
    ptr = k.ps("ptr", [128, D], BF16)

    zt = k.sb("zt", [128, 2, D], F32)
    k.op("pool", "memset", zt[:], 0.0, w=[zt])
    yp_v = io["s_ypart"].t.rearrange("(n p) d -> p n d", p=128)
    for i in range(0, 4 * TT // 128, 2):
        k.dma("act", yp_v[:, i:i + 2, :], zt[:, :, :], zt, io["s_ypart"], zt)
    k.dma("sp", g2_s[:], io["g2T"].t[l], None, g2_s, g2_s)
    k.dma("sp", wr[:], io["w_router"].t[l].rearrange("(k p) n -> p k n", p=128), None, wr, wr)
    wm_v = io["w_br_mla"].t[l].rearrange("(h p) n -> p h n", p=64)
    for h in range(8):
        k.dma("pool", wmla[:, h, :], wm_v[:, h, :], None, wmla, wmla)
    wn_v = io["w_br_na"].t[l].rearrange("(h p) n -> p h n", p=64)
    for h in range(4):
        k.dma("pool", wna[:, h, :], wn_v[:, h, :], None, wna, wna)
    wp_v = io["w_br_pool"].t[l].rearrange("(k p) n -> p k n", p=128)
    for kk in range(2):
        k.dma("pool", wpl[:, kk, :], wp_v[:, kk, :], None, wpl, wpl)
    wo_v = io["w_out"].t[l].rearrange("(k p) n -> p k n", p=128)
    for kk in range(8):
        k.dma("pool", wout[:, kk, :], wo_v[:, kk, :], None, wout, wout)
    mod = _mod_setup(k, io, l, 16, 24)
    k.op("dve", "tensor_scalar", Acoef[:], mod[:, 16:24, :], 1.0, None, ALU.add, r=[mod], w=[Acoef])
    k.op("dve", "tensor_tensor", Acoef[:], Acoef[:], g2_s[:, :].unsqueeze(2).to_broadcast([128, 8, 2]), ALU.mult,
         r=[Acoef, g2_s], w=[Acoef])

    hi = [0]
    tiles = ([TILES[4]] + TILES[:4]) if with_ctx else TILES[:4]
    xT_v = xT.t.rearrange("(k p) n -> p k n", p=128)
    g_v = gateT.t.rearrange("(k p) n -> p k n", p=128)
    for (c0, ncs) in tiles:
        mi = 1 if c0 == TL else 0
        k.dma("sp", a_t[:, :, :ncs], aT.t[:, :, c0:c0 + ncs].rearrange("h p n -> p h n"), None, a_t, a_t)
        k.dma("sp", b_t[:, :, :ncs], bT.t[:, :, c0:c0 + ncs].rearrange("h p n -> p h n"), None, b_t, b_t)
        k.dma("sp", p_t[:, :, :ncs], pT.t.rearrange("(k p) n -> p k n", p=128)[:, :, c0:c0 + ncs], None, p_t, p_t)
        for q in range(3):
            k.dma("sp", g_t[:, q * 8:(q + 1) * 8, :ncs], g_v[:, q * 8:(q + 1) * 8, c0:c0 + ncs], None, g_t, g_t)
        k.dma("sp", xt[:, :, :ncs], xT_v[:, :, c0:c0 + ncs], None, xt, xt)
        for j in range(8):
            js = slice(j * 128, (j + 1) * 128)
            for h in range(8):
                k.mm(pa, pa[:, :ncs], wmla, wmla[:, h, js], a_t, a_t[:, h, :ncs], h == 0, h == 7)
            for h in range(4):
                k.mm(pb_, pb_[:, :ncs], wna, wna[:, h, js], b_t, b_t[:, h, :ncs], h == 0, h == 3)
            for kk in range(2):
                k.mm(pp, pp[:, :ncs], wpl, wpl[:, kk, js], p_t, p_t[:, kk, :ncs], kk == 0, kk == 1)
            k.op("dve", "tensor_tensor", t1[:, :ncs], pa[:, :ncs], g_t[:, j, :ncs], ALU.mult, r=[pa, g_t], w=[t1])
            k.op("dve", "tensor_tensor", t2[:, :ncs], pb_[:, :ncs], g_t[:, 8 + j, :ncs], ALU.mult, r=[pb_, g_t], w=[t2])
            k.op("pool", "tensor_tensor", t1[:, :ncs], t1[:, :ncs], t2[:, :ncs], ALU.add, r=[t1, t2], w=[t1])
            k.op("dve", "tensor_tensor", t2[:, :ncs], pp[:, :ncs], g_t[:, 16 + j, :ncs], ALU.mult, r=[pp, g_t], w=[t2])
            k.op("pool", "tensor_tensor", mT[:, j, :ncs], t1[:, :ncs], t2[:, :ncs], ALU.add, r=[t1, t2], w=[mT])
        for i in range(8):
            p_ = po[i % 2]
            for j in range(8):
                k.mm(p_, p_[:, :ncs], wout, wout[:, j, i * 128:(i + 1) * 128], mT, mT[:, j, :ncs], j == 0, j == 7)
            k.op("dve", "scalar_tensor_tensor", xmid[:, i, :ncs], p_[:, :ncs], mod[:, i, mi:mi + 1], xt[:, i, :ncs],
                 ALU.mult, ALU.add, r=[p_, mod, xt], w=[xmid])
        k.dma("sp", o_xmidT.t.rearrange("(k p) n -> p k n", p=128)[:, :, c0:c0 + ncs], xmid[:, :, :ncs], xmid, o_xmidT, xmid)
        for kk in range(8):
            k.op("act", "activation", sq[:, :ncs], xmid[:, kk, :ncs], AF.Square, r=[xmid], w=[sq])
            k.mm(pss, pss[:, :ncs], ones, ones[:, :], sq, sq[:, :ncs], kk == 0, kk == 7)
        _rsqrt_mean(k, rstd, pss, float(D), ncs, lntmp)
        for kk in range(8):
            k.op("dve", "tensor_tensor", xn[:, :ncs], xmid[:, kk, :ncs], rstd[:, :ncs], ALU.mult, r=[xmid, rstd], w=[xn])
            k.op("act", "activation", h2f[:, kk, :ncs], xn[:, :ncs], AF.Identity,
                 bias=mod[:, 8 + kk, mi:mi + 1], scale=Acoef[:, kk, mi:mi + 1], r=[xn, mod, Acoef], w=[h2f])
        k.op("pool", "tensor_copy", h2b[:, :, :ncs], h2f[:, :, :ncs], r=[h2f], w=[h2b])
        for tq in range(ncs // 128):
            ts_ = slice(tq * 128, (tq + 1) * 128)
            lg = po[tq % 2]
            for kk in range(8):
                k.mm(lg, lg[:, 0:16], h2f, h2f[:, kk, ts_], wr, wr[:, kk, :], kk == 0, kk == 7)
            k.op("dve", "tensor_reduce", sm[:, 0:1], lg[:, 0:16], AX.X, ALU.max, r=[lg], w=[sm])
            k.op("dve", "tensor_scalar", sm[:, 1:2], sm[:, 0:1], -1.0, None, ALU.mult, r=[sm], w=[sm])
            k.op("act", "activation", ex[:, :], lg[:, 0:16], AF.Exp, bias=sm[:, 1:2], scale=1.0, r=[lg, sm], w=[ex])
            k.op("dve", "tensor_reduce", sm[:, 2:3], ex[:, :], AX.X, ALU.add, r=[ex], w=[sm])
            k.op("dve", "reciprocal", sm[:, 3:4], sm[:, 2:3], r=[sm], w=[sm])
            af_ = afft[hi[0] % 2]
            k.op("dve", "tensor_scalar", af_[:, :], ex[:, :], sm[:, 3:4], None, ALU.mult, r=[ex, sm], w=[af_])
            k.dma("sp", o_aff[c0 + tq * 128:c0 + (tq + 1) * 128, :], af_[:, :], af_, o_aff, af_)
            for kk in range(8):
                k.op("pe", "transpose", ptr[:, kk * 128:(kk + 1) * 128], h2b[:, kk, ts_], idb[:, :], r=[h2b, idb], w=[ptr])
            ht_ = htok[hi[0] % 2]
            hi[0] += 1
            k.op("act", "activation", ht_[:, :], ptr[:, :], AF.Copy, r=[ptr], w=[ht_])
            k.dma("sp", o_h2[c0 + tq * 128:c0 + (tq + 1) * 128, :], ht_[:, :], ht_, o_h2, ht_)
        k.S.fence("pool")
        k.cc("AllGather", ALU.bypass, o_h2, o_h2[c0:c0 + ncs, :], io["g_h2"], io["g_h2"][4 * c0:4 * c0 + 4 * ncs, :])
    k.end_phase()


FF = 2048
NBIS = 27
NE = 4


def emit_c(k, io, l, with_ctx):
    c = _common(k, io, need_id=True)
    ones, idb = c["ones"], c["idb"]
    h2_g, aff_g, ypart = io["g_h2"], io["g_aff"], io["s_ypart"]
    wg = k.sb("wg", [128, 8, FF], BF16)
    wu = k.sb("wu", [128, 8, FF], BF16)
    wd = k.sb("wd", [128, 16, D], BF16)
    xsT = k.sb("xsT", [128, 8, 1152], BF16)
    hdn = k.sb("hdn", [128, 16, 1152], BF16)
    ones64 = k.sb("ones64", [128, 64], F32)
    iot = k.sb("iot", [128, 1024], F32)
    pj = k.sb("pj", [128, 64, 4], F32)
    pjc = k.sb("pjc", [128, 2, 4], F32)
    trif = k.sb("trif", [128, 128], F32)
    trib = k.sb("trib", [128, 128], BF16)
    cbase = k.sb("cbase", [128, 2], F32)
    esel = k.sb("esel", [128, NE, 16], F32)
    Aall = k.sb("Aall", [128, 64, 16], F32)
    Acall = k.sb("Acall", [128, 2, 16], F32)
    Atmp = k.sb("Atmp", [128, 64, 16], F32)
    A = k.sb("A", [128, NE, 64], F32)
    Ac = k.sb("Ac", [128, NE, 2], F32)
    cmpL = k.sb("cmpL", [128, NE, 64], F32)
    cmpC = k.sb("cmpC", [128, NE, 2], F32)
    lo = k.sb("lo", [128, 8], F32)
    mid = k.sb("mid", [128, 8], F32)
    cnt = k.sb("cnt", [128, 8], F32)
    fs = k.sb("fs", [128, 8], F32)
    incl = k.sb("incl", [128, 64], F32)
    rowt = k.sb("rowt", [128, 1], BF16)
    offs = k.sb("offs", [128, 1], F32)
    key = k.sb("key", [128, 64], F32)
    R = k.sb("R", [128, 64, 7], BF16)
    r1 = k.sb("r1", [128, 64], F32)
    r2 = k.sb("r2", [128, 64], F32)
    OH = [k.sb("OH%d" % i, [128, 1024], BF16) for i in range(4)]
    sel = k.sb("sel", [128, 9, 8], F32)
    idxf = k.sb("idxf", [128, 9], F32)
    idx2 = k.sb("idx2", [128, 9], F32)
    idxi_all = k.sb("idxi", [128, NE, 9], I32)
    idxs_all = k.sb("idxs", [128, NE, 9], I32)
    gval_all = k.sb("gval", [128, NE, 9], F32)
    xs_tok = [k.sb("xs_tok%d" % i, [128, D], BF16) for i in range(2)]
    sg = [k.sb("sg%d" % i, [128, 512], F32) for i in range(2)]
    y_sb = [k.sb("y_sb%d" % i, [128, D], F32) for i in range(2)]
    prt = k.ps("prt")
    ptot = prt
    poff = prt
    psel = prt
    ptr = k.ps("ptr", [128, D], BF16)
    pa2 = [k.ps("pa0"), k.ps("pa1")]
    pu2 = [k.ps("pu0"), k.ps("pu1")]
    py = [k.ps("py0"), k.ps("py1")]

    k.op("dve", "memset", ones64[:], 1.0, w=[ones64])
    k.dma("sp", iot[:], io["c_iota"][:, :], None, iot, iot)
    k.dma("sp", pj[:], io["c_pj"][:, :, :], None, pj, pj)
    k.dma("sp", pjc[:], io["c_pjc"][:, :, :], None, pjc, pjc)
    k.dma("sp", trif[:], io["c_tri"][:, :], None, trif, trif)
    k.dma("sp", cbase[:], io["c_base"][:, :], None, cbase, cbase)
    k.dma("sp", esel[:], io["esel"][:, :, :], None, esel, esel)
    k.op("dve", "tensor_copy", trib[:], trif[:], r=[trif], w=[trib])
    for q in range(4):
        k.dma("sp", Aall[:, q * 16:(q + 1) * 16, :], aff_g[q * TT:q * TT + TL, :].rearrange("(p j) e -> p j e", j=16),
              None, Aall, Aall)
    if with_ctx:
        k.dma("sp", Acall[:, :, :], aff_g[TL:TT, :].rearrange("(p j) e -> p j e", j=2), None, Acall, Acall)
    else:
        k.op("dve", "memset", Acall[:], 0.0, w=[Acall])
    for e in range(NE):
        k.op("dve", "tensor_tensor", Atmp[:], Aall[:], esel[:, e, :].unsqueeze(1).to_broadcast([128, 64, 16]), ALU.mult,
             r=[Aall, esel], w=[Atmp])
        k.op("dve", "tensor_reduce", A[:, e, :], Atmp[:], AX.X, ALU.add, r=[Atmp], w=[A])
        k.op("dve", "tensor_tensor", Atmp[:, 0:2, :], Acall[:], esel[:, e, :].unsqueeze(1).to_broadcast([128, 2, 16]),
             ALU.mult, r=[Acall, esel], w=[Atmp])
        k.op("dve", "tensor_reduce", Ac[:, e, :], Atmp[:, 0:2, :], AX.X, ALU.add, r=[Atmp], w=[Ac])

    k.op("dve", "memset", lo[:], 0.0, w=[lo])
    for it in range(1, NBIS + 1):
        step = float(2.0 ** -it)
        k.op("dve", "tensor_scalar", mid[:], lo[:], step, None, ALU.add, r=[lo], w=[mid])
        k.op("dve", "tensor_tensor", cmpL[:], A[:], mid[:, 0:4].unsqueeze(2).to_broadcast([128, 4, 64]), ALU.is_ge,
             r=[A, mid], w=[cmpL])
        k.op("dve", "tensor_tensor", cmpC[:], Ac[:], mid[:, 4:8].unsqueeze(2).to_broadcast([128, 4, 2]), ALU.is_ge,
             r=[Ac, mid], w=[cmpC])
        k.op("dve", "tensor_reduce", cnt[:, 0:4], cmpL[:], AX.X, ALU.add, r=[cmpL], w=[cnt])
        k.op("dve", "tensor_reduce", cnt[:, 4:8], cmpC[:], AX.X, ALU.add, r=[cmpC], w=[cnt])
        k.mm(ptot, ptot[:, 96:104], ones, ones[:, :], cnt, cnt[:, :], True, True)
        k.op("dve", "tensor_scalar", fs[:, 0:4], ptot[:, 96:100], 1023.5, step, ALU.is_ge, ALU.mult, r=[ptot], w=[fs])
        k.op("dve", "tensor_scalar", fs[:, 4:8], ptot[:, 100:104], 31.5, step, ALU.is_ge, ALU.mult, r=[ptot], w=[fs])
        k.op("dve", "tensor_tensor", lo[:], lo[:], fs[:], ALU.add, r=[lo, fs], w=[lo])

    ohi = [0]
    xi = [0]
    yi = [0]

    def compact(avals, pj_t, thr_col, ncol, nslot_tiles, nsl, st0):
        k.op("dve", "tensor_scalar", key[:, :ncol], avals, lo[:, thr_col:thr_col + 1], None, ALU.is_ge,
             r=[A, Ac, lo], w=[key])
        k.op("dve", "tensor_tensor_scan", incl[:, :ncol], ones64[:, :ncol], key[:, :ncol], 0.0, ALU.mult, ALU.add,
             r=[ones64, key], w=[incl])
        k.op("dve", "tensor_copy", rowt[:, :], incl[:, ncol - 1:ncol], r=[incl], w=[rowt])
        k.mm(poff, poff[:, 112:113], trib, trib[:, :], rowt, rowt[:, :], True, True)
        k.op("dve", "tensor_copy", offs[:, :], poff[:, 112:113], r=[poff], w=[offs])
        k.op("dve", "tensor_scalar", incl[:, :ncol], incl[:, :ncol], offs[:, 0:1], None, ALU.add, r=[incl, offs], w=[incl])
        k.op("dve", "tensor_tensor", key[:, :ncol], key[:, :ncol], incl[:, :ncol], ALU.mult, r=[key, incl], w=[key])
        k.op("dve", "tensor_copy", R[:, :ncol, 0:4], pj_t[:, :ncol, :], r=[pj_t], w=[R])
        k.op("dve", "tensor_copy", R[:, :ncol, 4], avals, r=[A, Ac], w=[R])
        k.op("dve", "tensor_tensor", r1[:, :ncol], avals, R[:, :ncol, 4], ALU.subtract, r=[A, Ac, R], w=[r1])
        k.op("dve", "tensor_copy", R[:, :ncol, 5], r1[:, :ncol], r=[r1], w=[R])
        k.op("dve", "tensor_tensor", r2[:, :ncol], r1[:, :ncol], R[:, :ncol, 5], ALU.subtract, r=[r1, R], w=[r2])
        k.op("dve", "tensor_copy", R[:, :ncol, 6], r2[:, :ncol], r=[r2], w=[R])
        for j in range(ncol):
            oh = OH[ohi[0] % 4]
            ohi[0] += 1
            if nsl < 128:
                k.op("pool", "memset", oh[:, 0:128], 0.0, w=[oh])
            k.op("dve", "tensor_scalar", oh[:, 0:nsl], iot[:, 0:nsl], key[:, j:j + 1], None, ALU.is_equal,
                 r=[iot, key], w=[oh])
            for st in range(nslot_tiles):
                k.op("pe", "matmul", psel[:, (st0 + st) * 8:(st0 + st) * 8 + 7], oh[:, st * 128:(st + 1) * 128],
                     R[:, j, :], start=(j == 0 and st == 0), stop=(j == ncol - 1), skip_group_check=True,
                     r=[oh, R], w=[psel])

    def finish_sel(e, st0, nst, mg, ms, base_col):
        k.op("dve", "tensor_copy", sel[:, st0:st0 + nst, :], psel[:, st0 * 8:(st0 + nst) * 8].rearrange("p (s c) -> p s c", c=8), r=[psel], w=[sel])
        for (mults, dst_i, bcol) in ((mg, idxi_all, 0), (ms, idxs_all, 1)):
            k.op("dve", "tensor_scalar", idxf[:, st0:st0 + nst], sel[:, st0:st0 + nst, 0], float(mults[0]), None, ALU.mult, r=[sel], w=[idxf])
            for ci in (1, 2):
                k.op("dve", "scalar_tensor_tensor", idxf[:, st0:st0 + nst], sel[:, st0:st0 + nst, ci], float(mults[ci]), idxf[:, st0:st0 + nst],
                     ALU.mult, ALU.add, r=[sel, idxf], w=[idxf])
            k.op("dve", "tensor_tensor", idxf[:, st0:st0 + nst], idxf[:, st0:st0 + nst], sel[:, st0:st0 + nst, 3], ALU.add, r=[idxf, sel], w=[idxf])
            if base_col:
                k.op("dve", "tensor_scalar", idxf[:, st0:st0 + nst], idxf[:, st0:st0 + nst], cbase[:, bcol:bcol + 1], None, ALU.add,
                     r=[idxf, cbase], w=[idxf])
            k.op("dve", "tensor_copy", dst_i[:, e, st0:st0 + nst], idxf[:, st0:st0 + nst], r=[idxf], w=[dst_i])
        k.op("dve", "tensor_tensor", gval_all[:, e, st0:st0 + nst], sel[:, st0:st0 + nst, 4], sel[:, st0:st0 + nst, 5], ALU.add, r=[sel], w=[gval_all])
        k.op("dve", "tensor_tensor", gval_all[:, e, st0:st0 + nst], gval_all[:, e, st0:st0 + nst], sel[:, st0:st0 + nst, 6], ALU.add, r=[gval_all, sel], w=[gval_all])

    def gather_T(ex, st0, nst):
        for st in range(st0, st0 + nst):
            x_ = xs_tok[xi[0] % 2]
            xi[0] += 1

            def fn(e, x_=x_, st=st):
                return e.indirect_dma_start(out=x_[:, :], out_offset=None, in_=h2_g.t[:, :],
                                            in_offset=bass.IndirectOffsetOnAxis(ap=idxi_all[:, ex, st:st + 1], axis=0))
            k.S.dma2("pool", fn, idxi_all.b, x_.b, x_.b)
            for kk in range(8):
                k.op("pe", "transpose", ptr[:, kk * 128:(kk + 1) * 128], x_[:, kk * 128:(kk + 1) * 128], idb[:, :],
                     r=[x_, idb], w=[ptr])
            k.op("act", "activation", xsT[:, :, st * 128:(st + 1) * 128], ptr[:, :].rearrange("p (k s) -> p k s", s=128),
                 AF.Copy, r=[ptr], w=[xsT])

    first_scatter = [True]

    def gu(nst):
        ncols = nst * 128
        for fc in range(16):
            fs_ = slice(fc * 128, (fc + 1) * 128)
            for c0 in range(0, ncols, 512):
                nn = min(512, ncols - c0)
                pa = pa2[yi[0] % 2]
                pu = pu2[yi[0] % 2]
                for kk in range(8):
                    k.mm(pa, pa[:, :nn], wg, wg[:, kk, fs_], xsT, xsT[:, kk, c0:c0 + nn], kk == 0, kk == 7)
                for kk in range(8):
                    k.mm(pu, pu[:, :nn], wu, wu[:, kk, fs_], xsT, xsT[:, kk, c0:c0 + nn], kk == 0, kk == 7)
                s_ = sg[yi[0] % 2]
                yi[0] += 1
                k.op("act", "activation", s_[:, :nn], pa[:, :nn], AF.Silu, r=[pa], w=[s_])
                k.op("dve", "tensor_tensor", hdn[:, fc, c0:c0 + nn], pu[:, :nn], s_[:, :nn], ALU.mult, r=[pu, s_], w=[hdn])

    def down(ex, nst):
        for st in range(nst):
            y_ = y_sb[yi[0] % 2]
            yi[0] += 1
            for dh in range(2):
                p_ = py[dh]
                for fc in range(16):
                    k.mm(p_, p_[:, :512], hdn, hdn[:, fc, st * 128:(st + 1) * 128], wd, wd[:, fc, dh * 512:(dh + 1) * 512],
                         fc == 0, fc == 15)
                k.op("act", "activation", y_[:, dh * 512:(dh + 1) * 512], p_[:, :512], AF.Copy, scale=gval_all[:, ex, st:st + 1],
                     r=[p_, gval_all], w=[y_])

            def fn(e, y_=y_, st=st):
                return e.indirect_dma_start(out=ypart.t[:, :],
                                            out_offset=bass.IndirectOffsetOnAxis(ap=idxs_all[:, ex, st:st + 1], axis=0),
                                            in_=y_[:, :], in_offset=None, compute_op=ALU.add, oob_is_err=True)
            k.S.dma2("pool", fn, y_.b, ypart.b, y_.b)
            idxs_all.b.rd.append(ypart.b.w)

    def load_gu(e):
        wgv = io["w_gate4"].t[l, e].rearrange("(k p) n -> p k n", p=128)
        wuv = io["w_up4"].t[l, e].rearrange("(k p) n -> p k n", p=128)
        for kk in range(8):
            k.dma("pool", wg[:, kk, :], wgv[:, kk, :], None, wg, wg)
            k.dma("pool", wu[:, kk, :], wuv[:, kk, :], None, wu, wu)

    def load_d(e):
        wdv = io["w_down4"].t[l, e].rearrange("(k p) n -> p k n", p=128)
        for kk in range(16):
            k.dma("pool", wd[:, kk, :], wdv[:, kk, :], None, wd, wd)

    load_gu(0)
    load_d(0)
    nst_all = 9 if with_ctx else 8
    for e in range(NE):
        compact(A[:, e, :], pj, e, 64, 8, 1024, 0)
        finish_sel(e, 0, 8, (2048, 16, 512), (512, 16, TT), False)
        if with_ctx:
            compact(Ac[:, e, :], pjc, 4 + e, 2, 1, 32, 8)
            finish_sel(e, 8, 1, (2, 0, 0), (2, 0, 0), True)
    for e in range(NE):
        gather_T(e, 0, nst_all)
        gu(nst_all)
        if e + 1 < NE:
            load_gu(e + 1)
        down(e, nst_all)
        if e + 1 < NE:
            load_d(e + 1)
    k.end_phase()


def emit_d(k, io, l, last, o_xT):
    c = _common(k, io, need_id=True)
    ones, idf = c["ones"], c["idf"]
    ysum, xmidT = io["s_ysum"], io["s_xmidT"]
    fg_s = k.sb("fg_s", [128, 8], F32)
    yt = [k.sb("yt%d" % i, [128, D], F32) for i in range(4)]
    xm2 = [k.sb("xm%d" % i, [128, 8, 512], F32) for i in range(2)]
    xo2 = [k.sb("xo%d" % i, [128, 8, 512], F32) for i in range(2)]
    sq = k.sb("sq", [128, 512], F32)
    rstd = k.sb("rstd", [128, 512], F32)
    lntmp = k.sb("lntmp", [128, 512], F32)
    xn = k.sb("xn", [128, 512], F32)
    pss = k.ps("pss")
    ptr = [k.ps("ptr0", [128, D], F32), k.ps("ptr1", [128, D], F32)]
    k.dma("sp", fg_s[:], io["fgT"][:, :], None, fg_s, fg_s)
    mod = _mod_setup(k, io, l, 40, 8)
    si = [0]
    tiles = TILES[:4] if last else TILES
    xv = xmidT.t.rearrange("(k p) n -> p k n", p=128)
    for ti, (c0, ncs) in enumerate(tiles):
        mi = 1 if ti == 4 else 0
        xm = xm2[ti % 2]
        xo = xo2[ti % 2]
        k.dma("sp", xm[:, :, :ncs], xv[:, :, c0:c0 + ncs], None, xm, xm)
        for tq in range(ncs // 128):
            r0 = c0 + tq * 128
            y_ = yt[si[0] % 4]
            p_ = ptr[si[0] % 2]
            si[0] += 1
            k.dma("sp", y_[:, :], ysum[r0:r0 + 128, :], None, y_, y_)
            for kk in range(8):
                k.op("pe", "transpose", p_[:, kk * 128:(kk + 1) * 128], y_[:, kk * 128:(kk + 1) * 128], idf[:, :],
                     r=[y_, idf], w=[p_])
            for kk in range(8):
                k.op("dve", "scalar_tensor_tensor", xo[:, kk, tq * 128:(tq + 1) * 128], p_[:, kk * 128:(kk + 1) * 128],
                     mod[:, kk, mi:mi + 1], xm[:, kk, tq * 128:(tq + 1) * 128], ALU.mult, ALU.add,
                     r=[p_, mod, xm], w=[xo])
        if last:
            for kk in range(8):
                k.op("act", "activation", sq[:, :ncs], xo[:, kk, :ncs], AF.Square, r=[xo], w=[sq])
                k.mm(pss, pss[:, :ncs], ones, ones[:, :], sq, sq[:, :ncs], kk == 0, kk == 7)
            _rsqrt_mean(k, rstd, pss, float(D), ncs, lntmp)
            for kk in range(8):
                k.op("dve", "tensor_tensor", xn[:, :ncs], xo[:, kk, :ncs], rstd[:, :ncs], ALU.mult, r=[xo, rstd], w=[xn])
                k.op("act", "activation", xo[:, kk, :ncs], xn[:, :ncs], AF.Copy, scale=fg_s[:, kk:kk + 1],
                     r=[xn, fg_s], w=[xo])
        k.dma("sp", o_xT.t.rearrange("(k p) n -> p k n", p=128)[:, :, c0:c0 + ncs], xo[:, :, :ncs], xo, o_xT, xo)
    k.end_phase()


def build_fused(dbg=False):
    k = KB()
    io = {}

    def ein(name, shape, dt=F32):
        io[name] = k.din(name, shape, dt)

    ein("xT0", [D, TT]); ein("cT", [128, 8, 2]); ein("w_ada", [2, D, 6 * D]); ein("b_adaT", [2, 128, 48])
    ein("g1T", [2, 128, 8]); ein("g2T", [2, 128, 8]); ein("w_in", [2, D, DIN]); ein("gqT", [2, 128, 3])
    ein("gkvT", [2, 128, 2]); ein("w_uq", [2, 384, 768]); ein("w_ukv", [2, 256, 1024]); ein("ropeT", [32, 2, TL])
    ein("nbias", [2, 4, 128, 8, 384]); ein("pcinv", [128, 2, TT]); ein("w_pool", [2, 4, 64, 64])
    ein("pscaleT", [2, 128, 2]); ein("hmask", [128, 8]); ein("w_br_mla", [2, 512, D]); ein("w_br_na", [2, 256, D])
    ein("w_br_pool", [2, 256, D]); ein("w_out", [2, D, D]); ein("w_router", [2, D, 16]); ein("ident", [128, 128])
    ein("w_gate4", [2, NE, D, FF]); ein("w_up4", [2, NE, D, FF]); ein("w_down4", [2, NE, FF, D])
    ein("c_iota", [128, 1024]); ein("c_pj", [128, 64, 4]); ein("c_pjc", [128, 2, 4]); ein("c_tri", [128, 128])
    ein("c_base", [128, 2]); ein("esel", [128, NE, 16]); ein("fgT", [128, 8])
    io["out"] = k.dout("o_xT", [D, TL], F32)

    def scr(name, shape, dt):
        io[name] = k.dscr(name, shape, dt)

    scr("s_mod", [128, 48, 2], F32)
    scr("s_qT", [768, TT], BF16); scr("s_kT", [544, TT], BF16); scr("s_v", [TT, 512], BF16)
    scr("s_nqT", [256, TT], BF16); scr("s_nkT", [256, TT], BF16); scr("s_nv", [TT, 256], BF16)
    scr("s_poolT", [256, TT], F32); scr("s_gateT", [3072, TT], BF16)
    scr("g_kT", [4 * 544, TT], BF16); scr("g_v", [4 * TT, 512], BF16)
    scr("s_nkh", [256, NHK], BF16); scr("g_nkh", [4 * 256, NHK], BF16)
    scr("s_nvh", [NHK, 256], BF16); scr("g_nvh", [4 * NHK, 256], BF16)
    scr("s_ph", [256, 2 * HALO], F32); scr("g_ph", [4 * 256, 2 * HALO], F32)
    scr("s_kfr", [256, FR * GW], BF16); scr("s_vfr", [FR * GW, 256], BF16)
    scr("s_pfr", [256, TL + 2 * HALO], F32); scr("s_pcf", [256, NCTX + 2 * HALO], F32)
    scr("s_aT", [8, 64, TT], BF16); scr("s_bT", [4, 64, TT], BF16); scr("s_pT", [256, TT], BF16)
    scr("s_xmidT", [D, TT], F32); scr("s_h2", [TT, D], BF16); scr("s_aff", [TT, 16], F32)
    scr("g_h2", [4 * TT, D], BF16); scr("g_aff", [4 * TT, 16], F32)
    scr("s_ypart", [4 * TT, D], F32); scr("s_ysum", [TT, D], F32); scr("s_x1T", [D, TT], F32)

    for l in range(2):
        last = l == 1
        xT = io["xT0"] if l == 0 else io["s_x1T"]
        emit_mod(k, io, l)
        emit_a(k, io, l, xT)
        emit_frames(k, io)
        emit_b1(k, io, l, not last)
        emit_b2(k, io, l, xT, not last)
        k.cc("AllGather", ALU.bypass, io["s_aff"], io["s_aff"][:, :], io["g_aff"], io["g_aff"][:, :])
        k.end_phase()
        emit_c(k, io, l, not last)
        k.cc("ReduceScatter", ALU.add, io["s_ypart"], io["s_ypart"][:, :], io["s_ysum"], io["s_ysum"][:, :])
        k.end_phase()
        emit_d(k, io, l, last, io["out"] if last else io["s_x1T"])
        if dbg and l == 0:
            down = k.sb("down", [128, 1], F32)
            for nm in ("s_aT", "s_bT", "s_pT", "s_xmidT", "s_h2", "s_aff", "s_ysum", "s_x1T", "s_kfr", "s_vfr", "s_pfr"):
                src = io[nm]
                shp = [int(v) for v in src.t.shape]
                dst = k.dout("d_" + nm, shp, src.t.dtype)
                if len(shp) == 3:
                    k.dma("sp", dst[:, :, :], src[:, :, :], src, dst, down)
                else:
                    k.dma("sp", dst[:, :], src[:, :], src, dst, down)
            k.end_phase()
    k.S.finalize()
    return k.nc


def lay128(v):
    v = np.asarray(v)
    return np.ascontiguousarray(v.reshape(-1, 128).T)


def rope_tables(core):
    t0 = (core % 4) * TL
    t = np.arange(t0, t0 + TL)
    pos = [(t // GW).astype(np.float32), (t % GW).astype(np.float32)]
    inv = (np.float32(10000.0) ** (-np.arange(8, dtype=np.float32) / np.float32(8))).astype(np.float32)
    tab = np.zeros((32, 2, TL), np.float32)
    for i in range(32):
        ang = (pos[i // 16] * inv[(i % 16) % 8]).astype(np.float32)
        tab[i, 0] = np.cos(ang)
        tab[i, 1] = np.sin(ang)
    return tab


def na_bias_tables(rpb_all, core):
    rank = core % 4
    r0 = rank * 32
    qc = np.arange(GW)
    start_c = np.clip(qc - 8, 0, GW - 16)
    kcol = np.arange(GW)
    colmask = (kcol[:, None] >= start_c[None, :]) & (kcol[:, None] < start_c[None, :] + 16)
    dc = np.clip(kcol[:, None] - qc[None, :], -15, 15) + 15
    out = np.full((2, 4, 128, 8, 384), -30000.0, np.float32)
    rl_of_var = {0: 10, 1: 0, 2: 1, 3: 2, 4: 3, 5: 29, 6: 30, 7: 31}
    for l in range(2):
        rpb = np.asarray(rpb_all[l])
        for var in range(8):
            rl = rl_of_var[var]
            s0, nch, v_ = na_window(rl)
            assert v_ == var
            g = r0 + rl
            sr = int(np.clip(g - 4, 0, 128 - 8))
            for i in range(nch):
                for hf in range(2):
                    gr = r0 - 7 + s0 + 2 * i + hf
                    if not (sr <= gr < sr + 8):
                        continue
                    dr = gr - g + 7
                    blk = np.where(colmask[None], rpb[:, dr][:, dc], np.float32(-30000.0))
                    out[l, :, hf * 64:(hf + 1) * 64, var, i * 64:(i + 1) * 64] = blk
    return out


def moe_consts():
    c_iota = np.broadcast_to(np.arange(1, 1025, dtype=np.float32)[None, :], (128, 1024)).copy()
    jj = np.arange(64)
    pp = np.arange(128)
    c_pj = np.zeros((128, 64, 4), np.float32)
    c_pj[:, :, 0] = (pp // 32).astype(np.float32)[:, None]
    c_pj[:, :, 1] = (pp % 32).astype(np.float32)[:, None]
    c_pj[:, :, 2] = (jj // 16).astype(np.float32)[None, :]
    c_pj[:, :, 3] = (jj % 16).astype(np.float32)[None, :]
    c_pjc = np.zeros((128, 2, 4), np.float32)
    c_pjc[:, :, 0] = pp.astype(np.float32)[:, None]
    c_pjc[:, :, 3] = np.arange(2, dtype=np.float32)[None, :]
    c_tri = (pp[:, None] < pp[None, :]).astype(np.float32)
    c_base = np.zeros((128, 2), np.float32)
    c_base[0:32, 0] = 4 * TL
    c_base[32:128, 0] = 4 * TL + NCTX + np.arange(96)
    c_base[0:32, 1] = TL
    c_base[32:128, 1] = TT + TL + np.arange(96)
    return c_iota, c_pj, c_pjc, c_tri, c_base


def host_inputs(inp):
    c_iota, c_pj, c_pjc, c_tri, c_base = moe_consts()
    ident = np.eye(128, dtype=np.float32)
    st2 = lambda key: np.ascontiguousarray(np.stack([lay128(inp[key][l]) for l in range(2)], axis=0))
    shared = {
        "w_ada": inp["w_ada"], "b_adaT": st2("b_ada"), "g1T": st2("norm1_g"), "g2T": st2("norm2_g"),
        "w_in": inp["w_in"], "gqT": st2("mla_q_norm"), "gkvT": st2("mla_kv_norm"), "w_uq": inp["w_uq"],
        "w_ukv": inp["w_ukv"], "w_pool": inp["w_pool"], "pscaleT": st2("pool_scale"),
        "w_br_mla": inp["w_br_mla"], "w_br_na": inp["w_br_na"], "w_br_pool": inp["w_br_pool"],
        "w_out": inp["w_out"], "w_router": inp["w_router"], "ident": ident,
        "c_iota": c_iota, "c_pj": c_pj, "c_pjc": c_pjc, "c_tri": c_tri, "c_base": c_base,
        "fgT": lay128(inp["final_g"]),
    }
    maps = []
    for core in range(NCORE):
        b, rank = core // 4, core % 4
        t0 = rank * TL
        m = dict(shared)
        m["xT0"] = np.ascontiguousarray(np.concatenate([inp["x"][b, t0:t0 + TL], inp["ctx"][b]], axis=0).T)
        m["cT"] = np.ascontiguousarray(np.stack([lay128(inp["c"][b]), lay128(inp["c_ctx"])], axis=-1))
        m["ropeT"] = rope_tables(core)
        m["nbias"] = na_bias_tables(inp["na_rpb"], core)
        pcinv = np.zeros((128, 2, TT), np.float32)
        for g, w in enumerate((2, 4, 8, 16)):
            c_, hf = g // 2, g % 2
            t = np.arange(t0, t0 + TL)
            cnt = np.clip(t + w // 2, 0, NSEQ) - np.clip(t - w // 2, 0, NSEQ)
            tcx = np.arange(NCTX)
            cntc = np.clip(tcx + w // 2, 0, NCTX) - np.clip(tcx - w // 2, 0, NCTX)
            pcinv[hf * 64:(hf + 1) * 64, c_, :TL] = (np.float32(1.0) / cnt.astype(np.float32))[None, :]
            pcinv[hf * 64:(hf + 1) * 64, c_, TL:] = (np.float32(1.0) / cntc.astype(np.float32))[None, :]
        m["pcinv"] = pcinv
        hm = np.zeros((128, 8), np.float32)
        if rank > 0:
            hm[:, rank - 1] = 1.0
        if rank < 3:
            hm[:, 4 + rank + 1] = 1.0
        m["hmask"] = hm
        es = np.zeros((128, NE, 16), np.float32)
        for i in range(NE):
            es[:, i, NE * rank + i] = 1.0
        m["esel"] = es
        m["w_gate4"] = np.ascontiguousarray(inp["w_gate"][:, NE * rank:NE * rank + NE])
        m["w_up4"] = np.ascontiguousarray(inp["w_up"][:, NE * rank:NE * rank + NE])
        m["w_down4"] = np.ascontiguousarray(inp["w_down"][:, NE * rank:NE * rank + NE])
        maps.append(m)
    return maps


_NC = []
_DBG = [False, None]


def kernel(**inputs):
    inp = {k_: np.asarray(v) for k_, v in inputs.items()}
    if not _NC:
        _NC.append(build_fused(_DBG[0]))
    res = run_bass_kernel_spmd(_NC[0], host_inputs(inp), core_ids=list(range(NCORE)))
    if _DBG[0]:
        _DBG[1] = res.results
    out = np.empty((NB, NSEQ, D), np.float32)
    for core in range(NCORE):
        b, rank = core // 4, core % 4
        out[b, rank * TL:(rank + 1) * TL] = np.asarray(res.results[core]["o_xT"]).T
    return out
```
